# Optimizing a Trainium2 kernel written in Bass

```python
import jax, jax.numpy as jnp
from jax import lax
import numpy as np

D_MODEL = 2048
BATCH = 8
SEQ = 2048
DEPTH = 1

D_MIX = D_MODEL
D_LRU = D_MIX // 2
D_SGU = D_MIX - D_LRU
LRU_HEADS = 8
LRU_BLOCK = D_LRU // LRU_HEADS
CONV_WIDTH = 4
LRU_C = 8.0
SGU_GROUPS = 8
SGU_GROUP_DIM = D_SGU // SGU_GROUPS
CHUNK = 128
PEER_HEADS = 8
N_KEYS = 128
N_EXPERTS = N_KEYS * N_KEYS
PEER_TOPK = 16
D_KEY = 256
PEER_BLOCK = 128
EPS = 1e-6

kernel_name = "hybrid_rglru_sgu_peer_adaln_block"


def rms_norm(x, g):
    xf = x.astype(jnp.float32)
    y = xf * lax.rsqrt(jnp.mean(xf * xf, axis=-1, keepdims=True) + EPS)
    return (y * g.astype(jnp.float32)).astype(x.dtype)


def modulate(h, shift, scale):
    return h * (1 + scale[:, None, :]) + shift[:, None, :]


def causal_depthwise_conv(x, w, b):
    out = lax.conv_general_dilated(
        x, w[:, None, :].astype(x.dtype), window_strides=(1,),
        padding=[(CONV_WIDTH - 1, 0)],
        dimension_numbers=("NWC", "WIO", "NWC"),
        feature_group_count=x.shape[-1])
    return out + b


def rg_lru(x, w_a, b_a, w_i, b_i, lam):
    B, S, _ = x.shape
    xh = x.reshape(B, S, LRU_HEADS, LRU_BLOCK)
    r = jax.nn.sigmoid(jnp.einsum('bshi,hij->bshj', xh, w_a) + b_a).reshape(B, S, D_LRU)
    i = jax.nn.sigmoid(jnp.einsum('bshi,hij->bshj', xh, w_i) + b_i).reshape(B, S, D_LRU)
    log_a = -LRU_C * r.astype(jnp.float32) * jax.nn.softplus(-lam.astype(jnp.float32))
    a = jnp.exp(log_a)
    b = jnp.sqrt(-jnp.expm1(2.0 * log_a)) * (i * x).astype(jnp.float32)

    def combine(left, right):
        a1, b1 = left
        a2, b2 = right
        return a1 * a2, a2 * b1 + b2

    _, h = lax.associative_scan(combine, (a, b), axis=1)
    return h.astype(x.dtype)


def spatial_gating(u, v, g_v, w_s, b_s):
    B, S, _ = v.shape
    nc = S // CHUNK
    v = rms_norm(v, g_v)
    vv = v.reshape(B, nc, CHUNK, SGU_GROUPS, SGU_GROUP_DIM)
    mask = jnp.tril(jnp.ones((CHUNK, CHUNK), dtype=bool))
    w = jnp.where(mask[None], w_s, 0)
    s = jnp.einsum('gij,bnjgd->bnigd', w, vv) + b_s.T[None, None, :, :, None]
    return u * s.reshape(B, S, D_SGU)


def hybrid_mixer(h, w_in, conv_w, conv_b, w_gate_a, b_gate_a, w_gate_i, b_gate_i,
                 lru_lambda, g_v, w_spatial, b_spatial, w_out):
    proj = h @ w_in
    x_lru, y_gate, u, v = jnp.split(proj, [D_LRU, 2 * D_LRU, 2 * D_LRU + D_SGU], axis=-1)
    xc = causal_depthwise_conv(x_lru, conv_w, conv_b)
    y_rec = rg_lru(xc, w_gate_a, b_gate_a, w_gate_i, b_gate_i, lru_lambda) * jax.nn.gelu(y_gate)
    y_sgu = spatial_gating(jax.nn.gelu(u), jax.nn.gelu(v), g_v, w_spatial, b_spatial)
    return jnp.concatenate([y_rec, y_sgu], axis=-1) @ w_out


def peer(h, w_query, sub_keys, expert_u, expert_v):
    B, S, D = h.shape
    q = (h @ w_query).reshape(B, S, PEER_HEADS, 2, D_KEY // 2)
    s = jnp.einsum('bshpk,hpnk->bshpn', q, sub_keys).astype(jnp.float32)
    v1, i1 = lax.top_k(s[..., 0, :], PEER_TOPK)
    v2, i2 = lax.top_k(s[..., 1, :], PEER_TOPK)
    n_cand = PEER_TOPK * PEER_TOPK
    cand = (v1[..., :, None] + v2[..., None, :]).reshape(B, S, PEER_HEADS, n_cand)
    cand_idx = (i1[..., :, None] * N_KEYS + i2[..., None, :]).reshape(B, S, PEER_HEADS, n_cand)
    top_v, pos = lax.top_k(cand, PEER_TOPK)
    idx = jnp.take_along_axis(cand_idx, pos, axis=-1)
    g = jax.nn.softmax(top_v, axis=-1)
    n_sel = PEER_HEADS * PEER_TOPK
    nb = (B * S) // PEER_BLOCK
    xb = h.reshape(nb, PEER_BLOCK, D)
    ib = idx.reshape(nb, PEER_BLOCK, n_sel)
    gb = g.astype(h.dtype).reshape(nb, PEER_BLOCK, n_sel)

    def block(args):
        xt, it, gt = args
        u = jnp.take(expert_u, it, axis=0)
        act = jax.nn.gelu(jnp.einsum('td,ted->te', xt, u)) * gt
        vv = jnp.take(expert_v, it, axis=0)
        return jnp.einsum('te,ted->td', act, vv)

    out = lax.map(block, (xb, ib, gb))
    return out.reshape(B, S, D)


def setup_inputs(seed: int = 0) -> dict:
    key = jax.random.key(seed)
    k = jax.random.split(key, 24)
    f32 = jnp.float32

    def nrm(kk, shape, scale):
        return jax.random.normal(kk, shape, f32) * scale

    L = DEPTH
    a0 = jax.random.uniform(k[10], (L, D_LRU), f32, minval=0.9, maxval=0.999)
    return {
        "x": nrm(k[0], (BATCH, SEQ, D_MODEL), 1.0),
        "c": nrm(k[1], (BATCH, D_MODEL), 1.0),
        "w_ada": nrm(k[2], (L, D_MODEL, 6 * D_MODEL), 0.5 * D_MODEL ** -0.5),
        "b_ada": nrm(k[3], (L, 6 * D_MODEL), 0.01),
        "g_norm_mix": 1.0 + nrm(k[4], (L, D_MODEL), 0.02),
        "w_in": nrm(k[5], (L, D_MODEL, 2 * D_LRU + 2 * D_SGU), D_MODEL ** -0.5),
        "conv_w": nrm(k[6], (L, CONV_WIDTH, D_LRU), CONV_WIDTH ** -0.5),
        "conv_b": nrm(k[7], (L, D_LRU), 0.01),
        "w_gate_a": nrm(k[8], (L, LRU_HEADS, LRU_BLOCK, LRU_BLOCK), LRU_BLOCK ** -0.5),
        "b_gate_a": nrm(k[9], (L, LRU_HEADS, LRU_BLOCK), 0.01),
        "w_gate_i": nrm(k[11], (L, LRU_HEADS, LRU_BLOCK, LRU_BLOCK), LRU_BLOCK ** -0.5),
        "b_gate_i": nrm(k[12], (L, LRU_HEADS, LRU_BLOCK), 0.01),
        "lru_lambda": jnp.log(a0) - jnp.log1p(-a0),
        "g_v": 1.0 + nrm(k[13], (L, D_SGU), 0.02),
        "w_spatial": nrm(k[14], (L, SGU_GROUPS, CHUNK, CHUNK), 0.5 * CHUNK ** -0.5),
        "b_spatial": 1.0 + nrm(k[15], (L, SGU_GROUPS, CHUNK), 0.01),
        "w_out": nrm(k[16], (L, D_MIX, D_MODEL), D_MIX ** -0.5),
        "g_norm_ffn": 1.0 + nrm(k[17], (L, D_MODEL), 0.02),
        "w_query": nrm(k[18], (L, D_MODEL, PEER_HEADS * D_KEY), D_MODEL ** -0.5),
        "sub_keys": nrm(k[19], (L, PEER_HEADS, 2, N_KEYS, D_KEY // 2), (D_KEY // 2) ** -0.5),
        "expert_u": nrm(k[20], (L, N_EXPERTS, D_MODEL), D_MODEL ** -0.5),
        "expert_v": nrm(k[21], (L, N_EXPERTS, D_MODEL), 1.0),
        "g_final": 1.0 + nrm(k[22], (D_MODEL,), 0.02),
    }


def reference(x, c, w_ada, b_ada, g_norm_mix, w_in, conv_w, conv_b, w_gate_a, b_gate_a,
              w_gate_i, b_gate_i, lru_lambda, g_v, w_spatial, b_spatial, w_out,
              g_norm_ffn, w_query, sub_keys, expert_u, expert_v, g_final):
    for l in range(DEPTH):
        mod = jax.nn.silu(c) @ w_ada[l] + b_ada[l]
        sh_m, sc_m, gt_m, sh_f, sc_f, gt_f = jnp.split(mod, 6, axis=-1)
        h = modulate(rms_norm(x, g_norm_mix[l]), sh_m, sc_m)
        mix = hybrid_mixer(h, w_in[l], conv_w[l], conv_b[l], w_gate_a[l], b_gate_a[l],
                           w_gate_i[l], b_gate_i[l], lru_lambda[l], g_v[l],
                           w_spatial[l], b_spatial[l], w_out[l])
        x = x + gt_m[:, None, :] * mix
        h = modulate(rms_norm(x, g_norm_ffn[l]), sh_f, sc_f)
        x = x + gt_f[:, None, :] * peer(h, w_query[l], sub_keys[l], expert_u[l], expert_v[l])
    return rms_norm(x, g_final)
```

```python
import contextlib
import numpy as np
import concourse.bass as bass
import concourse.mybir as mybir
from concourse.bass_utils import run_bass_kernel_spmd

F32 = mybir.dt.float32
BF16 = mybir.dt.bfloat16
I32 = mybir.dt.int32
U32 = mybir.dt.uint32
AF = mybir.ActivationFunctionType
ALU = mybir.AluOpType

P = 128
D = 2048
SEQ = 2048
NT = SEQ // P
NDC = D // P
NEXP = 16384
EPS = 1e-6
GW = 256
NEG = -1.0e30
TAIL_SKEW = 3
FRONT_SPAN = 118

DEBUG_TILES = None
DEBUG_CORES = 8
DEBUG_NCH = 8
DEBUG_SLOT = True
DEBUG_STAGE = 0


class Prog:
    def __init__(self, nc, es):
        self.nc = nc
        self.eng = {"pe": nc.tensor, "act": nc.scalar, "dve": nc.vector, "pool": nc.gpsimd, "sp": nc.sync}
        self.semh = {}
        for k in self.eng:
            self.semh[k] = es.enter_context(nc.semaphore("sem_" + k))
        self.cnt = {k: 0 for k in self.eng}
        self.waited = {k: {} for k in self.eng}
        self.lastw = {}
        self.readers = {}
        self.dpools = {}
        self.dval = {}
        self.es = es

    def add_dma_pool(self, name, n):
        names = []
        for i in range(n):
            nm = "d_%s%d" % (name, i)
            self.semh[nm] = self.es.enter_context(self.nc.semaphore(nm))
            self.dval[nm] = 0
            names.append(nm)
        self.dpools[name] = [names, 0]

    def _wait(self, e, tok):
        nm, val = tok
        w = self.waited[e]
        if w.get(nm, 0) >= val:
            return
        self.eng[e].wait_ge(self.semh[nm], val)
        w[nm] = val

    def _deps(self, e, reads, writes):
        deps = set()
        for k in reads:
            t = self.lastw.get(k)
            if t is not None:
                deps.add(t)
        for k in writes:
            t = self.lastw.get(k)
            if t is not None:
                deps.add(t)
            for nm, val in self.readers.get(k, {}).items():
                deps.add((nm, val))
        for t in sorted(deps):
            if e == "pe" and t[0] == "pe":
                continue
            self._wait(e, t)

    def _record(self, tok, reads, writes):
        for k in writes:
            self.lastw[k] = tok
            self.readers[k] = {}
        for k in reads:
            r = self.readers.setdefault(k, {})
            if r.get(tok[0], 0) < tok[1]:
                r[tok[0]] = tok[1]

    def op(self, e, fn, reads=(), writes=()):
        self._deps(e, reads, writes)
        ins = fn(self.eng[e])
        self.cnt[e] += 1
        ins.then_inc(self.semh[e], 1)
        tok = (e, self.cnt[e])
        self._record(tok, reads, writes)
        return tok

    def dma(self, q, pool, fn, reads=(), writes=()):
        names, rr = self.dpools[pool]
        nm = names[rr % len(names)]
        self.dpools[pool][1] = rr + 1
        self._deps(q, reads, writes)
        if self.dval[nm] > 0:
            self._wait(q, (nm, self.dval[nm]))
        ins = fn(self.eng[q])
        self.dval[nm] += 16
        ins.then_inc(self.semh[nm], 16)
        tok = (nm, self.dval[nm])
        self._record(tok, reads, writes)
        return tok


def build_program():
    nc = bass.Bass("TRN2", target_bir_lowering=False)
    ntiles = NT if DEBUG_TILES is None else DEBUG_TILES

    def din(name, shape, dt=F32):
        return nc.dram_tensor(name, list(shape), dt, kind="ExternalInput").ap()

    x_d = din("x", [SEQ, D])
    c_d = din("c16", [16, P])
    wada_d = din("w_ada", [D, 6 * D])
    bada_d = din("b_ada", [1, 6 * D])
    g1_d = din("g1row", [1, D])
    g2_d = din("g2row", [1, D])
    gf_d = din("gf_b", [P, D])
    gv_d = din("gv_b", [P, 1024])
    win_d = din("w_in", [D, 4096])
    wout_d = din("w_out", [D, D])
    wq_d = din("w_query", [D, D])
    cw_d = din("cw", [P, 8, 4])
    cb_d = din("cb", [P, 8])
    wa_d = din("wa", [8, P, P])
    wi_d = din("wi", [8, P, P])
    ba_d = din("ba", [P, 8])
    bi_d = din("bi", [P, 8])
    lam_d = din("lam", [P, 8])
    wsT_d = din("wsT", [P, 8, P])
    bs_d = din("bsrow", [1, 1024])
    kT_d = din("keysT", [P, 16, P])
    eu_d = din("expert_u", [NEXP, D])
    ev_d = din("expert_v", [NEXP, D])
    out_d = nc.dram_tensor("out", [SEQ, D], F32, kind="ExternalOutput").ap()
    euv_d = nc.dram_tensor("euv_bf16", [NEXP, 2 * D], BF16, kind="Internal").ap()
    winb_d = nc.dram_tensor("win_bf16", [4096, D], BF16, kind="Internal").ap()
    woutb_d = nc.dram_tensor("wout_bf16", [D, D], BF16, kind="Internal").ap()
    wqb_d = nc.dram_tensor("wq_bf16", [D, D], BF16, kind="Internal").ap()

    wada_v = wada_d.rearrange("(kc k) n -> k kc n", k=P)
    win_v = win_d.rearrange("(kc k) n -> k kc n", k=P)
    wout_v = wout_d.rearrange("(kc k) n -> k kc n", k=P)
    win_rows = win_d.rearrange("r (h n) -> (r h) n", h=2)
    winb_v = winb_d.rearrange("(r h) n -> r (h n)", h=2).rearrange("(kc k) n -> k kc n", k=P)
    woutb_v = woutb_d.rearrange("(kc k) n -> k kc n", k=P)
    wq_v = wq_d.rearrange("(kc k) n -> k kc n", k=P)
    wqb_v = wqb_d.rearrange("(kc k) n -> k kc n", k=P)

    with contextlib.ExitStack() as es:
        es.enter_context(nc.allow_low_precision("bf16 expert-value accumulation with fp32 PSUM"))

        def sb(name, shape, dt=F32):
            return es.enter_context(nc.sbuf_tensor("s_" + name, list(shape), dt))

        def ps(name, shape, dt=F32):
            return es.enter_context(nc.psum_tensor("p_" + name, list(shape), dt))

        A2 = sb("A2", [P, D]); B2 = sb("B2", [P, D])
        GF = sb("GF", [P, D]); GV = sb("GV", [P, 1024])
        keysT = sb("keysT", [P, 16, P]); wa = sb("wa", [P, 8, P]); wi = sb("wi", [P, 8, P]); WmT = sb("WmT", [P, 8, P])
        ident = sb("ident", [P, P]); identb = sb("identb", [P, P], BF16)
        ones_row = sb("ones_row", [1, P]); bsrow = sb("bsrow", [1, 1024])
        cw = sb("cw", [P, 8, 4]); cb = sb("cb", [P, 8]); nba = sb("nba", [P, 8]); nbi = sb("nbi", [P, 8])
        lam = sb("lam", [P, 8]); cch = sb("cch", [P, 8]); cch2 = sb("cch2", [P, 8])
        A1col = sb("A1col", [P, 16]); B1col = sb("B1col", [P, 16]); scol = sb("scol", [P, 16]); c16 = sb("c16", [16, P])
        state = sb("state", [P, 8]); epsc = sb("epsc", [P, 1])
        wbuf = [sb("wbuf0", [P, NDC, GW]), sb("wbuf1", [P, NDC, GW])]
        xt = [sb("xt0", [P, D]), sb("xt1", [P, D])]; hn = [sb("hn0", [P, D]), sb("hn1", [P, D])]
        junk = sb("junk", [P, D], BF16)
        xl = sb("xl", [P, 8, P + 3]); ycat = sb("ycat", [P, NDC, P], BF16); hTb = sb("hTb", [P, NDC, P], BF16)
        tmpg = [sb("tmpg0", [P, P]), sb("tmpg1", [P, P])]
        vn = sb("vn", [P, 1024])
        NR = 2
        xc = [sb("xc%d" % i, [P, P]) for i in range(NR)]
        rt = [sb("rt%d" % i, [P, P]) for i in range(NR)]
        it = [sb("it%d" % i, [P, P]) for i in range(NR)]
        at = [sb("at%d" % i, [P, P]) for i in range(NR)]
        a2t = [sb("a2t%d" % i, [P, P]) for i in range(NR)]
        hs = [sb("hs%d" % i, [P, P]) for i in range(NR)]
        qTs = [sb("qTs%d" % i, [P, P]) for i in range(NR)]
        stt = sb("stt", [P, 16]); sttb = sb("sttb", [P, 16])
        v12 = sb("v12", [P, 2, 16]); iu = sb("iu", [P, 2, 16], U32); if12 = sb("if12", [P, 2, 16])
        tmpk = sb("tmpk", [P, P]); cand = sb("cand", [P, 16, 16]); cand2 = sb("cand2", [P, 256]); cidx = sb("cidx", [P, 16, 16])
        j256 = cand2; tv = sb("tv", [P, 16]); ew = sb("ew", [P, 16]); tks = sb("tks", [P, 4])
        idxf = sb("idxf", [P, P]); idxi = [sb("idxi0", [P, P], I32), sb("idxi1", [P, P], I32)]; gw = [sb("gw0", [P, P]), sb("gw1", [P, P])]
        zt = sb("zt", [P, P]); actt = sb("actt", [P, P])
        NCB = 6
        CB = [sb("CB%d" % i, [P, 2 * D], BF16) for i in range(NCB)]
        GTM = CB[4][:, :].bitcast(F32)
        GTF = CB[5][:, :].bitcast(F32)
        kGTM = ["modtile2", ("CBu", 4), ("CBv", 4)]
        kGTF = ["modtile5", ("CBu", 5), ("CBv", 5)]
        Hb = sb("hb", [P, D], BF16)
        Dg = [sb("Dg0", [P, P], BF16), sb("Dg1", [P, P], BF16)]
        big = ps("big", [P, D])
        yacc = ps("yacc", [P, D])

        pr = Prog(nc, es)
        pr.add_dma_pool("w", 4)
        pr.add_dma_pool("m", 6)
        pr.add_dma_pool("g", 8)

        BIG = [("big", j) for j in range(4)]
        HT = [("hT", j) for j in range(NDC)]
        YC = [("ycat", j) for j in range(NDC)]
        XL = [("xl", j) for j in range(8)]

        wseq = []
        for g in range(6 * D // GW):
            wseq.append((wada_v, g, False))
        per_tile = [(winb_v, g, True) for g in (0, 1, 2, 3, 6, 7, 4, 5)] + [(woutb_v, g, True) for g in range(4)] + \
                   [(wqb_v, g, True) for g in range(4)]
        for _ in range(ntiles):
            wseq.extend(per_tile)
        wstate = {"issued": 0, "used": 0}

        def w_issue_upto(n):
            while wstate["issued"] <= n and wstate["issued"] < len(wseq):
                i = wstate["issued"]
                view, g, isb = wseq[i]
                slot = i % 2
                if isb:
                    pr.dma("sp", "w", lambda e, view=view, g=g, slot=slot: e.dma_start(
                        out=wbuf[slot][:, :, :].bitcast(BF16), in_=view[:, :, g * 512:(g + 1) * 512]), reads=(), writes=[("w", slot)])
                else:
                    pr.dma("sp", "w", lambda e, view=view, g=g, slot=slot: e.dma_start(
                        out=wbuf[slot][:, :, :], in_=view[:, :, g * GW:(g + 1) * GW]), reads=(), writes=[("w", slot)])
                wstate["issued"] += 1

        def w_next():
            i = wstate["used"]
            w_issue_upto(i)
            wstate["used"] += 1
            wb_ = wbuf[i % 2][:, :, :].bitcast(BF16) if wseq[i][2] else wbuf[i % 2]
            return wb_, ("w", i % 2), i

        def w_prefetch():
            w_issue_upto(wstate["used"])

        def mload(dst, src, key):
            pr.dma("sp", "m", lambda e: e.dma_start(out=dst, in_=src), reads=(), writes=[key])

        mload(cw[:, :, :], cw_d[:, :, :], "cw"); mload(cb[:, :], cb_d[:, :], "cb")
        mload(nba[:, :], ba_d[:, :], "nba"); mload(nbi[:, :], bi_d[:, :], "nbi"); mload(lam[:, :], lam_d[:, :], "lam")
        mload(wa[:, :, :], wa_d.rearrange("h i j -> i h j"), "wa"); mload(wi[:, :, :], wi_d.rearrange("h i j -> i h j"), "wi")
        mload(WmT[:, :, :], wsT_d[:, :, :], "WmT")
        mload(bsrow[:, :], bs_d[:, :], "bsrow"); mload(c16[:, :], c_d[:, :], "c16")
        mload(GF[:, :], gf_d[:, :], "GF"); mload(GV[:, :], gv_d[:, :], "GV")
        w_prefetch()

        pr.op("pool", lambda e: e.memset(ident[:, :], 1.0), writes=["ident"])
        pr.op("pool", lambda e: e.affine_select(out=ident[:, :], in_=ident[:, :], pattern=[[1, P]], compare_op=ALU.is_equal,
                                                fill=0.0, base=0, channel_multiplier=-1), reads=["ident"], writes=["ident"])
        pr.op("pool", lambda e: e.tensor_copy(out=identb[:, :], in_=ident[:, :]), reads=["ident"], writes=["identb"])
        pr.op("pool", lambda e: e.memset(ones_row[:, :], 1.0), writes=["ones_row"])
        pr.op("pool", lambda e: e.memset(state[:, :], 0.0), writes=["state"])
        pr.op("pool", lambda e: e.memset(epsc[:, :], EPS), writes=["epsc"])
        pr.op("pool", lambda e: e.memset(xl[:, :, :], 0.0), writes=XL + ["xlh"])
        pr.op("pool", lambda e: e.affine_select(out=WmT[:, :, :], in_=WmT[:, :, :], pattern=[[0, 8], [1, P]], compare_op=ALU.is_ge,
                                                fill=0.0, base=0, channel_multiplier=-1), reads=["WmT"], writes=["WmT"])
        pr.op("dve", lambda e: e.tensor_scalar(out=nba[:, :], in0=nba[:, :], scalar1=-1.0, scalar2=None, op0=ALU.mult), reads=["nba"], writes=["nba"])
        pr.op("dve", lambda e: e.tensor_scalar(out=nbi[:, :], in0=nbi[:, :], scalar1=-1.0, scalar2=None, op0=ALU.mult), reads=["nbi"], writes=["nbi"])
        pr.op("act", lambda e: e.activation(out=cch[:, :], in_=lam[:, :], func=AF.Exp, scale=-1.0), reads=["lam"], writes=["cch"])
        pr.op("act", lambda e: e.activation(out=cch[:, :], in_=cch[:, :], func=AF.Ln, bias=1.0, scale=1.0), reads=["cch"], writes=["cch"])
        pr.op("dve", lambda e: e.tensor_scalar(out=cch2[:, :], in0=cch[:, :], scalar1=-16.0, scalar2=None, op0=ALU.mult), reads=["cch"], writes=["cch2"])
        pr.op("dve", lambda e: e.tensor_scalar(out=cch[:, :], in0=cch[:, :], scalar1=-8.0, scalar2=None, op0=ALU.mult), reads=["cch", "cch2"], writes=["cch"])
        pr.op("pe", lambda e: e.transpose(out=big[:, 512:528], in_=c16[:, :], identity=ident[0:16, 0:16]), reads=["c16", "ident"], writes=[("big", 1)])
        pr.op("act", lambda e: e.activation(out=scol[:, :], in_=big[:, 512:528], func=AF.Silu), reads=[("big", 1)], writes=["scol"])

        stage = [(xt[0], "xt0"), (xt[1], "xt1"), (hn[0], "hn0"), (hn[1], "hn1")]
        jobs = [(win_rows, winb_d, i, None) for i in range(4096 // P)] + [(wq_d, wqb_d, i, None) for i in range(D // P)] + \
               [(eu_d, euv_d[:, 0:D], i, None) for i in range(NEXP // P)]
        NPLAIN = len(jobs)
        jobs += [(wout_d, woutb_d, i, "m") for i in range(D // P)]
        jobsV = [(ev_d, euv_d[:, D:2 * D], i, "f") for i in range(NEXP // P)]

        def conv_gen(jobs, stage, LOOK):
            NS = len(stage)
            for i in range(len(jobs) + LOOK):
                if i < len(jobs):
                    src, _, r, _ = jobs[i]
                    st_, sk_ = stage[i % NS]
                    pr.dma("sp", "m", lambda e: e.dma_start(out=st_[:, :], in_=src[r * P:(r + 1) * P, :]), writes=[sk_])
                j = i - LOOK
                if j >= 0:
                    _, dst, r, sc_ = jobs[j]
                    st_, sk_ = stage[j % NS]
                    gb_, gk_ = CB[j % 4][:, 0:D], ("CBu", j % 4)
                    ce = ("act", "dve", "pool")[j % 3]
                    if sc_ is not None:
                        gt_, kgt_ = (GTM, kGTM) if sc_ == "m" else (GTF, kGTF)
                        pr.op("dve", lambda e: e.scalar_tensor_tensor(out=gb_[:, :], in0=st_[:, :], scalar=1.0, op0=ALU.mult, in1=gt_[:, :], op1=ALU.mult),
                              reads=[sk_] + kgt_, writes=[gk_])
                    elif ce == "act":
                        pr.op("act", lambda e: e.activation(out=gb_[:, :], in_=st_[:, :], func=AF.Copy), reads=[sk_], writes=[gk_])
                    else:
                        pr.op(ce, lambda e: e.tensor_copy(out=gb_[:, :], in_=st_[:, :]), reads=[sk_], writes=[gk_])
                    pr.dma("sp", "m", lambda e: e.dma_start(out=dst[r * P:(r + 1) * P, :], in_=gb_[:, :]), reads=[gk_], writes=[])
                yield i

        rowt = GTF[0:1, :]
        g1row = B2[0:1, :]
        g2row = A2[0:1, :]
        badarow = keysT[0:1, :, :].rearrange("p a b -> p (a b)")
        kROW, kG1, kG2, kBA = "modtile5", "modtile3", "modtile4", "keysT"

        def adaln_gen():
            mload(g1row, g1_d[:, :], kG1)
            mload(g2row, g2_d[:, :], kG2)
            for v in range(6):
                pr.dma("sp", "m", lambda e, v=v: e.dma_start(out=badarow, in_=bada_d[:, v * D:(v + 1) * D]), writes=[kBA])
                for g in range(D // GW):
                    wb, wkey, _ = w_next()
                    if wstate["used"] < 6 * D // GW:
                        w_prefetch()
                    for kc in range(NDC):
                        pr.op("pe", lambda e, kc=kc, g=g, wb=wb: e.matmul(big[0:1, g * GW:(g + 1) * GW], lhsT=scol[:, kc:kc + 1], rhs=wb[:, kc, :],
                                                                        start=(kc == 0), stop=(kc == NDC - 1)),
                              reads=["scol", wkey], writes=[("big", g // 2)])
                    yield
                pr.op("dve", lambda e: e.tensor_tensor(out=rowt, in0=big[0:1, :], in1=badarow, op=ALU.add), reads=BIG + [kBA], writes=[kROW])
                if v == 1:
                    pr.op("dve", lambda e: e.scalar_tensor_tensor(out=rowt, in0=rowt, scalar=1.0, op0=ALU.add, in1=g1row, op1=ALU.mult),
                          reads=[kROW, kG1], writes=[kROW])
                if v == 4:
                    pr.op("dve", lambda e: e.scalar_tensor_tensor(out=rowt, in0=rowt, scalar=1.0, op0=ALU.add, in1=g2row, op1=ALU.mult),
                          reads=[kROW, kG2], writes=[kROW])
                if v in (0, 1):
                    for dc in range(NDC):
                        pr.op("pe", lambda e, dc=dc: e.matmul(big[:, 1024 + 2 * dc:1024 + 2 * dc + 2], lhsT=rowt[0:1, dc * P:(dc + 1) * P], rhs=ones_row[0:1, 0:2],
                                                            start=True, stop=True), reads=[kROW, "ones_row"], writes=[("big", 2)])
                    dst = B1col if v == 0 else A1col
                    pr.op("act", lambda e, dst=dst: e.activation(out=dst[:, :], in_=big[:, 1024:1056].rearrange("p (a b) -> p a b", b=2)[:, :, 0], func=AF.Copy),
                          reads=[("big", 2)], writes=["B1col" if v == 0 else "A1col"])
                else:
                    dst = {2: GTM, 3: B2, 4: A2, 5: GTF}[v]
                    for j in range(4):
                        pr.op("pe", lambda e, j=j: e.matmul(big[:, j * 512:(j + 1) * 512], lhsT=ones_row[0:1, :], rhs=rowt[0:1, j * 512:(j + 1) * 512],
                                                          start=True, stop=True), reads=[kROW, "ones_row"], writes=[("big", j)])
                    pr.op("act", lambda e, dst=dst: e.activation(out=dst[:, :], in_=big[:, :], func=AF.Copy), reads=BIG,
                          writes=(kGTM if v == 2 else kGTF if v == 5 else ["modtile%d" % v]))
                yield

        gc_, ga_ = conv_gen(jobs, stage, 3), adaln_gen()
        dc_ = da_ = False
        it_ = 0
        while not (dc_ and da_):
            it_ += 1
            if not dc_:
                try:
                    ji_ = next(gc_)
                    if ji_ + 1 >= NPLAIN:
                        for _ in ga_:
                            pass
                        da_ = True
                except StopIteration:
                    dc_ = True
            if not da_ and (dc_ or it_ % 3 == 0):
                try:
                    next(ga_)
                except StopIteration:
                    da_ = True
        for nm_ in pr.dpools["m"][0]:
            if pr.dval[nm_] > 0:
                pr._wait("sp", (nm_, pr.dval[nm_]))
        mload(keysT[:, :, :], kT_d[:, :, :], "keysT")
        w_prefetch()

        MT = ["modtile3", "modtile4"]

        def rstd_from_ssq(ssq_col, out_col, n, key_in, key_out):
            pr.op("act", lambda e: e.activation(out=out_col, in_=ssq_col, func=AF.Ln, scale=1.0 / n, bias=epsc[:, 0:1]), reads=[key_in, "epsc"], writes=[key_out])
            pr.op("act", lambda e: e.activation(out=out_col, in_=out_col, func=AF.Exp, scale=-0.5), reads=[key_out], writes=[key_out])

        out_toks = []

        def front(ti):
            p = ti % 2
            X, H = xt[p], hn[p]
            kX, kH, kI, kG = "xt%d" % p, "hn%d" % p, "idxi%d" % p, "gw%d" % p
            r0 = ti * P
            pr.dma("sp", "m", lambda e: e.dma_start(out=X[:, :], in_=x_d[r0:r0 + P, :]), writes=[kX])
            yield
            pr.op("act", lambda e: e.activation(out=junk[:, :], in_=X[:, :], func=AF.Square, accum_out=stt[:, 0:1]),
                  reads=[kX], writes=["junk", "ssq1"])
            rstd_from_ssq(stt[:, 0:1], stt[:, 1:2], D, "ssq1", "rstd1")
            pr.op("act", lambda e: e.activation(out=H[:, :], in_=X[:, :], func=AF.Copy, scale=stt[:, 1:2]), reads=[kX, "rstd1"], writes=[kH])
            yield
            for dc in range(NDC):
                pr.op("pe", lambda e, dc=dc: e.transpose(out=big[:, dc * P:(dc + 1) * P], in_=H[:, dc * P:(dc + 1) * P], identity=ident[:, :]),
                      reads=[kH, "ident"], writes=[("big", dc // 4)])
            yield
            for dc in range(NDC):
                pr.op("act", lambda e, dc=dc: e.activation(out=hTb[:, dc, :], in_=big[:, dc * P:(dc + 1) * P], func=AF.Identity,
                                                          scale=A1col[:, dc:dc + 1], bias=B1col[:, dc:dc + 1]),
                      reads=[("big", dc // 4), "A1col", "B1col"], writes=[("hTb", dc)])
            yield
            if ti > 0:
                pr.op("dve", lambda e: e.tensor_copy(out=xl[:, :, 0:3], in_=xl[:, :, P:P + 3]), reads=XL, writes=["xlh"])

            cur = {}

            def fm_chunk(c24, bank, evac):
                if c24 % 4 == 0:
                    cur["w"] = w_next()
                    w_prefetch()
                wb, wkey, _ = cur["w"]
                for dc in range(NDC):
                    pr.op("pe", lambda e, dc=dc: e.matmul(big[:, bank * 512:bank * 512 + P], lhsT=wb[:, dc, (c24 % 4) * P:(c24 % 4 + 1) * P], rhs=hTb[:, dc, :],
                                                        start=(dc == 0), stop=(dc == NDC - 1)),
                          reads=[wkey, ("hTb", dc)], writes=[("big", bank)])
                evac(big[:, bank * 512:bank * 512 + P], ("big", bank))

            for c in range(8):
                fm_chunk(c, c % 2, lambda src, key, c=c: pr.op("act", lambda e: e.activation(out=xl[:, c, 3:3 + P], in_=src, func=AF.Copy),
                                                                reads=[key], writes=[("xl", c)]))
                yield

            def lru_chunk(c):
                k = c % NR
                Xc, R, I, A, A2t, Hs = xc[k], rt[k], it[k], at[k], a2t[k], hs[k]
                kx, kr, ki, ka, ka2, kh = ("xc", k), ("rt", k), ("it", k), ("at", k), ("a2t", k), ("hs", k)
                pr.op("dve", lambda e: e.tensor_scalar(out=Xc[:, :], in0=xl[:, c, 3:3 + P], scalar1=cw[:, c, 3:4], scalar2=cb[:, c:c + 1],
                                                       op0=ALU.mult, op1=ALU.add), reads=[("xl", c), "cw", "cb"], writes=[kx])
                for kk in (2, 1, 0):
                    pr.op("dve", lambda e, kk=kk: e.scalar_tensor_tensor(out=Xc[:, :], in0=xl[:, c, kk:kk + P], scalar=cw[:, c, kk:kk + 1], op0=ALU.mult,
                                                                          in1=Xc[:, :], op1=ALU.add), reads=[("xl", c), "xlh", "cw", kx], writes=[kx])
                yield
                gbk = 2 + c % 2
                gk_ = ("big", gbk)
                gR = big[:, gbk * 512:gbk * 512 + P]
                gI = big[:, gbk * 512 + P:gbk * 512 + 2 * P]
                pr.op("pe", lambda e: e.matmul(gR, lhsT=wa[:, c, :], rhs=Xc[:, :], start=True, stop=True), reads=["wa", kx], writes=[gk_])
                pr.op("pe", lambda e: e.matmul(gI, lhsT=wi[:, c, :], rhs=Xc[:, :], start=True, stop=True), reads=["wi", kx], writes=[gk_])
                yield
                pr.op("act", lambda e: e.activation(out=R[:, :], in_=gR, func=AF.Exp, scale=-1.0, bias=nba[:, c:c + 1]), reads=[gk_, "nba"], writes=[kr])
                pr.op("act", lambda e: e.activation(out=I[:, :], in_=gI, func=AF.Exp, scale=-1.0, bias=nbi[:, c:c + 1]), reads=[gk_, "nbi"], writes=[ki])
                yield
                pr.op("dve", lambda e: e.tensor_scalar(out=R[:, :], in0=R[:, :], scalar1=1.0, scalar2=None, op0=ALU.add), reads=[kr], writes=[kr])
                pr.op("dve", lambda e: e.reciprocal(out=R[:, :], in_=R[:, :]), reads=[kr], writes=[kr])
                pr.op("dve", lambda e: e.tensor_scalar(out=I[:, :], in0=I[:, :], scalar1=1.0, scalar2=None, op0=ALU.add), reads=[ki], writes=[ki])
                pr.op("dve", lambda e: e.reciprocal(out=I[:, :], in_=I[:, :]), reads=[ki], writes=[ki])
                pr.op("dve", lambda e: e.tensor_tensor(out=I[:, :], in0=I[:, :], in1=Xc[:, :], op=ALU.mult), reads=[ki, kx], writes=[ki])
                yield
                pr.op("act", lambda e: e.activation(out=A[:, :], in_=R[:, :], func=AF.Exp, scale=cch[:, c:c + 1]), reads=[kr, "cch"], writes=[ka])
                pr.op("act", lambda e: e.activation(out=A2t[:, :], in_=R[:, :], func=AF.Exp, scale=cch2[:, c:c + 1]), reads=[kr, "cch2"], writes=[ka2])
                yield
                pr.op("dve", lambda e: e.tensor_scalar(out=A2t[:, :], in0=A2t[:, :], scalar1=-1.0, scalar2=1.0, op0=ALU.mult, op1=ALU.add),
                      reads=[ka2], writes=[ka2])
                pr.op("dve", lambda e: e.tensor_scalar(out=A2t[:, :], in0=A2t[:, :], scalar1=1e-30, scalar2=None, op0=ALU.max), reads=[ka2], writes=[ka2])
                yield
                pr.op("act", lambda e: e.activation(out=A2t[:, :], in_=A2t[:, :], func=AF.Ln), reads=[ka2], writes=[ka2])
                pr.op("act", lambda e: e.activation(out=A2t[:, :], in_=A2t[:, :], func=AF.Exp, scale=0.5), reads=[ka2], writes=[ka2])
                yield
                pr.op("dve", lambda e: e.tensor_tensor(out=I[:, :], in0=I[:, :], in1=A2t[:, :], op=ALU.mult), reads=[ki, ka2], writes=[ki])
                pr.op("dve", lambda e: e.tensor_tensor_scan(out=Hs[:, :], data0=A[:, :], data1=I[:, :], initial=state[:, c:c + 1],
                                                            op0=ALU.mult, op1=ALU.add), reads=[ka, ki, "state"], writes=[kh])
                pr.op("dve", lambda e: e.tensor_copy(out=state[:, c:c + 1], in_=Hs[:, P - 1:P]), reads=[kh], writes=["state"])
                pr.op("dve", lambda e: e.tensor_copy(out=ycat[:, c, :], in_=Hs[:, :]), reads=[kh], writes=[("ycat", c)])
                yield

            for c0 in range(0, 8, 2):
                ga, gb2 = lru_chunk(c0), lru_chunk(c0 + 1)
                for _ in zip(ga, gb2):
                    yield
            pend = None

            def y_mul(c):
                tg, ktg = tmpg[c % 2], ("tmpg", c % 2)
                pr.op("dve", lambda e: e.tensor_tensor(out=ycat[:, c, :], in0=ycat[:, c, :], in1=tg[:, :], op=ALU.mult),
                      reads=[("ycat", c), ktg], writes=[("ycat", c)])

            for c in range(8):
                tg, ktg = tmpg[c % 2], ("tmpg", c % 2)
                if pend is not None and c >= 2:
                    pass
                fm_chunk(8 + c, c % 2, lambda src, key: pr.op("act", lambda e: e.activation(out=tg[:, :], in_=src, func=AF.Gelu),
                                                               reads=[key], writes=[ktg]))
                yield
                y_mul(c)
            yield
            for g2 in range(2):
                wb, wkey, _ = w_next()
                w_prefetch()
                for dc in range(NDC):
                    pr.op("pe", lambda e, dc=dc: e.matmul(big[:, g2 * 512:(g2 + 1) * 512], lhsT=hTb[:, dc, :], rhs=wb[:, dc, :],
                                                        start=(dc == 0), stop=(dc == NDC - 1)),
                          reads=[wkey, ("hTb", dc)], writes=[("big", g2)])
                yield
            pr.op("act", lambda e: e.activation(out=vn[:, :], in_=big[:, 0:1024], func=AF.Gelu), reads=[("big", 0), ("big", 1)], writes=["vn"])
            pr.op("act", lambda e: e.activation(out=junk[:, 0:1024], in_=vn[:, :], func=AF.Square, accum_out=stt[:, 2:3]),
                  reads=["vn"], writes=["junk", "ssqv"])
            rstd_from_ssq(stt[:, 2:3], stt[:, 3:4], 1024, "ssqv", "rstdv")
            yield
            pr.op("dve", lambda e: e.scalar_tensor_tensor(out=vn[:, :], in0=vn[:, :], scalar=stt[:, 3:4], op0=ALU.mult, in1=GV[:, :], op1=ALU.mult),
                  reads=["vn", "rstdv", "GV"], writes=["vn"])
            yield

            def sgu_mul(g):
                tg, ktg = tmpg[g % 2], ("tmpg", g % 2)
                sbk = 2 + g % 2
                pr.op("dve", lambda e: e.tensor_tensor(out=ycat[:, 8 + g, :], in0=tg[:, :], in1=big[:, sbk * 512:sbk * 512 + P], op=ALU.mult),
                      reads=[ktg, ("big", sbk)], writes=[("ycat", 8 + g)])

            for g in range(8):
                tg, ktg = tmpg[g % 2], ("tmpg", g % 2)
                fm_chunk(16 + g, g % 2, lambda src, key: pr.op("act", lambda e: e.activation(out=tg[:, :], in_=src, func=AF.Gelu),
                                                                reads=[key], writes=[ktg]))
                sbk = 2 + g % 2
                sk_ = ("big", sbk)
                sT = big[:, sbk * 512:sbk * 512 + P]
                pr.op("pe", lambda e: e.matmul(sT, lhsT=vn[:, g * P:(g + 1) * P], rhs=WmT[:, g, :], start=True, stop=False),
                      reads=["vn", "WmT"], writes=[sk_])
                pr.op("pe", lambda e: e.matmul(sT, lhsT=ones_row[0:1, :], rhs=bsrow[0:1, g * P:(g + 1) * P], start=False, stop=True),
                      reads=["ones_row", "bsrow"], writes=[sk_])
                yield
                sgu_mul(g)
            yield
            for g in range(4):
                wb, wkey, _ = w_next()
                w_prefetch()
                for fc in range(NDC):
                    pr.op("pe", lambda e, fc=fc: e.matmul(big[:, g * 512:(g + 1) * 512], lhsT=ycat[:, fc, :], rhs=wb[:, fc, :],
                                                        start=(fc == 0), stop=(fc == NDC - 1)),
                          reads=[wkey, ("ycat", fc)], writes=[("big", g)])
                yield
            pr.op("dve", lambda e: e.tensor_tensor(out=X[:, :], in0=X[:, :], in1=big[:, :], op=ALU.add), reads=[kX] + BIG, writes=[kX])
            yield
            pr.op("act", lambda e: e.activation(out=junk[:, :], in_=X[:, :], func=AF.Square, accum_out=stt[:, 4:5]),
                  reads=[kX], writes=["junk", "ssq2"])
            rstd_from_ssq(stt[:, 4:5], stt[:, 5:6], D, "ssq2", "rstd2")
            yield
            pr.op("dve", lambda e: e.scalar_tensor_tensor(out=H[:, :], in0=X[:, :], scalar=stt[:, 5:6], op0=ALU.mult, in1=A2[:, :], op1=ALU.mult),
                  reads=[kX, "rstd2"] + MT, writes=[kH])
            pr.op("dve", lambda e: e.tensor_tensor(out=H[:, :], in0=H[:, :], in1=B2[:, :], op=ALU.add), reads=[kH] + MT, writes=[kH])
            yield
            for dc in range(NDC):
                pr.op("pe", lambda e, dc=dc: e.transpose(out=big[:, dc * P:(dc + 1) * P], in_=H[:, dc * P:(dc + 1) * P], identity=ident[:, :]),
                      reads=[kH, "ident"], writes=[("big", dc // 4)])
            yield
            for j in range(4):
                pr.op("act", lambda e, j=j: e.activation(out=hTb[:, 4 * j:4 * j + 4, :], in_=big[:, j * 512:(j + 1) * 512].rearrange("p (a b) -> p a b", b=P),
                                                        func=AF.Copy), reads=[("big", j)], writes=[("hTb", 4 * j + i) for i in range(4)])
            yield

            def head_scores(h):
                if h % 2 == 0:
                    cur["wq"] = w_next()
                    w_prefetch()
                wb, wkey, _ = cur["wq"]
                sbk = 2 + h % 2
                kb = ("big", sbk)
                for p_ in range(2):
                    hp = 2 * h + p_
                    qb_ = big[:, p_ * 512:p_ * 512 + P]
                    qk_ = ("big", p_)
                    for dc in range(NDC):
                        co = ((h % 2) * 2 + p_) * P
                        pr.op("pe", lambda e, dc=dc: e.matmul(qb_, lhsT=wb[:, dc, co:co + P], rhs=hTb[:, dc, :],
                                                            start=(dc == 0), stop=(dc == NDC - 1)),
                              reads=[wkey, ("hTb", dc)], writes=[qk_])
                    q = qTs[p_]
                    pr.op("act", lambda e: e.activation(out=q[:, :], in_=qb_, func=AF.Copy), reads=[qk_], writes=[("qTs", p_)])
                    yield
                    pr.op("pe", lambda e: e.matmul(big[:, sbk * 512 + p_ * P:sbk * 512 + (p_ + 1) * P], lhsT=q[:, :], rhs=keysT[:, hp, :], start=True, stop=True),
                          reads=[("qTs", p_), "keysT"], writes=[kb])

            def head_topk(h):
                sbk = 2 + h % 2
                kb = ("big", sbk)
                for p_ in range(2):
                    sP = big[:, sbk * 512 + p_ * P:sbk * 512 + (p_ + 1) * P]
                    pr.op("dve", lambda e: e.max(out=v12[:, p_, 0:8], in_=sP), reads=[kb], writes=["v12"])
                    pr.op("dve", lambda e: e.max_index(out=iu[:, p_, 0:8], in_max=v12[:, p_, 0:8], in_values=sP), reads=[kb, "v12"], writes=["iu"])
                    pr.op("dve", lambda e: e.match_replace(out=tmpk[:, :], in_to_replace=v12[:, p_, 0:8], in_values=sP, imm_value=NEG),
                          reads=[kb, "v12"], writes=["tmpk"])
                    pr.op("dve", lambda e: e.max(out=v12[:, p_, 8:16], in_=tmpk[:, :]), reads=["tmpk"], writes=["v12"])
                    pr.op("dve", lambda e: e.max_index(out=iu[:, p_, 8:16], in_max=v12[:, p_, 8:16], in_values=tmpk[:, :]), reads=["tmpk", "v12"], writes=["iu"])
                    yield
                pr.op("dve", lambda e: e.tensor_copy(out=if12[:, :, :], in_=iu[:, :, :]), reads=["iu"], writes=["if12"])
                pr.op("dve", lambda e: e.tensor_tensor(out=cand[:, :, :], in0=v12[:, 0, :, None].broadcast_to([P, 16, 16]),
                                                       in1=v12[:, 1, None, :].broadcast_to([P, 16, 16]), op=ALU.add), reads=["v12"], writes=["cand"])
                pr.op("dve", lambda e: e.scalar_tensor_tensor(out=cidx[:, :, :], in0=if12[:, 0, :, None].broadcast_to([P, 16, 16]), scalar=float(P), op0=ALU.mult,
                                                              in1=if12[:, 1, None, :].broadcast_to([P, 16, 16]), op1=ALU.add), reads=["if12"], writes=["cidx"])
                candf = cand[:, :, :].rearrange("p a b -> p (a b)")
                cidxf = cidx[:, :, :].rearrange("p a b -> p (a b)")
                pr.op("dve", lambda e: e.max(out=tv[:, 0:8], in_=candf), reads=["cand"], writes=["tv"])
                pr.op("dve", lambda e: e.match_replace(out=cand2[:, :], in_to_replace=tv[:, 0:8], in_values=candf, imm_value=NEG),
                      reads=["cand", "tv"], writes=["cand2"])
                pr.op("dve", lambda e: e.max(out=tv[:, 8:16], in_=cand2[:, :]), reads=["cand2"], writes=["tv"])
                pr.op("dve", lambda e: e.tensor_scalar(out=tks[:, 0:1], in0=tv[:, 0:1], scalar1=-1.0, scalar2=None, op0=ALU.mult), reads=["tv"], writes=["tks0"])
                pr.op("act", lambda e: e.activation(out=ew[:, :], in_=tv[:, :], func=AF.Exp, bias=tks[:, 0:1], scale=1.0, accum_out=tks[:, 1:2]),
                      reads=["tv", "tks0"], writes=["ew", "tks1"])
                yield
                for k in range(16):
                    pr.op("dve", lambda e, k=k: e.scalar_tensor_tensor(out=j256[:, :], in0=candf, scalar=tv[:, k:k + 1], op0=ALU.is_equal, in1=cidxf, op1=ALU.mult,
                                                                        accum_out=idxf[:, h * 16 + k:h * 16 + k + 1]),
                          reads=["cand", "cidx", "tv"], writes=["cand2", ("idxf", h * 16 + k)])
                    if k % 4 == 3:
                        yield
                pr.op("dve", lambda e: e.reciprocal(out=tks[:, 2:3], in_=tks[:, 1:2]), reads=["tks1"], writes=["tks2"])
                pr.op("dve", lambda e: e.tensor_scalar(out=gw[p][:, h * 16:(h + 1) * 16], in0=ew[:, :], scalar1=tks[:, 2:3], scalar2=None, op0=ALU.mult),
                      reads=["ew", "tks2"], writes=[kG])

            for _ in head_scores(0):
                yield
            for h in range(8):
                gs_ = head_scores(h + 1) if h + 1 < 8 else iter(())
                gt_ = head_topk(h)
                done_s = done_t = False
                while not (done_s and done_t):
                    if not done_s:
                        try:
                            next(gs_)
                        except StopIteration:
                            done_s = True
                    if not done_t:
                        try:
                            next(gt_)
                        except StopIteration:
                            done_t = True
                    yield
            IDXF = [("idxf", i) for i in range(P)]
            pr.op("dve", lambda e: e.tensor_scalar(out=idxf[:, :], in0=idxf[:, :], scalar1=float(NEXP - 1), scalar2=0.0, op0=ALU.min, op1=ALU.max),
                  reads=IDXF, writes=IDXF)
            pr.op("dve", lambda e: e.tensor_copy(out=idxi[p][:, :], in_=idxf[:, :]), reads=IDXF, writes=[kI])
            yield

        gcnt = [0]

        def back(ti):
            p = ti % 2
            X, H = xt[p], hn[p]
            kX, kH, kI, kG = "xt%d" % p, "hn%d" % p, "idxi%d" % p, "gw%d" % p
            r0 = ti * P
            pr.op("act", lambda e: e.activation(out=Hb[:, :], in_=H[:, :], func=AF.Copy), reads=[kH], writes=["hb"])
            yield

            def head(s_, b):
                pr.dma("pool", "g", lambda e: e.indirect_dma_start(out=CB[b][:, :], out_offset=None, in_=euv_d[:, :],
                                                                   in_offset=bass.IndirectOffsetOnAxis(ap=idxi[p][:, s_:s_ + 1], axis=0)),
                       reads=[kI], writes=[("CBu", b), ("CBv", b)])
                pr.op("dve", lambda e: e.tensor_tensor(out=CB[b][:, 0:D], in0=CB[b][:, 0:D], in1=Hb[:, :], op=ALU.mult),
                      reads=[("CBu", b), "hb"], writes=[("CBu", b)])
                pr.op("act", lambda e: e.activation(out=CB[b][:, 0:D], in_=CB[b][:, 0:D], func=AF.Copy, accum_out=zt[:, s_:s_ + 1]),
                      reads=[("CBu", b)], writes=[("CBu", b), ("zt", s_)])
                pr.op("act", lambda e: e.activation(out=actt[:, s_:s_ + 1], in_=zt[:, s_:s_ + 1], func=AF.Gelu),
                      reads=[("zt", s_)], writes=[("actt", s_)])

            def tail(s_, b):
                d2 = s_ % 2
                pr.op("dve", lambda e: e.tensor_scalar(out=Dg[d2][:, :], in0=identb[:, :], scalar1=actt[:, s_:s_ + 1], scalar2=gw[p][:, s_:s_ + 1],
                                                       op0=ALU.mult, op1=ALU.mult),
                      reads=["identb", ("actt", s_), kG], writes=[("Dg", d2)])
                for j in range(4):
                    pr.op("pe", lambda e, j=j: e.matmul(yacc[:, j * 512:(j + 1) * 512], lhsT=Dg[d2][:, :], rhs=CB[b][:, D + j * 512:D + (j + 1) * 512],
                                                      start=(s_ == 0), stop=(s_ == P - 1)),
                          reads=[("Dg", d2), ("CBv", b)], writes=[("yacc", j)])

            pend = []
            for s_ in range(P):
                b = gcnt[0] % NCB
                gcnt[0] += 1
                head(s_, b)
                pend.append((s_, b))
                if len(pend) > TAIL_SKEW:
                    tail(*pend.pop(0))
                yield
            while pend:
                tail(*pend.pop(0))
            YA = [("yacc", j) for j in range(4)]
            pr.op("dve", lambda e: e.tensor_tensor(out=X[:, :], in0=X[:, :], in1=yacc[:, :], op=ALU.add), reads=[kX] + YA, writes=[kX])
            jb = gcnt[0] % NCB
            gcnt[0] += 1
            pr.op("act", lambda e: e.activation(out=CB[jb][:, 0:D], in_=X[:, :], func=AF.Square, accum_out=sttb[:, 0:1]),
                  reads=[kX], writes=[("CBu", jb), "ssq3"])
            rstd_from_ssq(sttb[:, 0:1], sttb[:, 1:2], D, "ssq3", "rstd3")
            pr.op("dve", lambda e: e.scalar_tensor_tensor(out=H[:, :], in0=X[:, :], scalar=sttb[:, 1:2], op0=ALU.mult, in1=GF[:, :], op1=ALU.mult),
                  reads=[kX, "rstd3", "GF"], writes=[kH])
            out_toks.append(pr.dma("sp", "m", lambda e: e.dma_start(out=out_d[r0:r0 + P, :], in_=H[:, :]), reads=[kH], writes=[]))
            yield

        nfront = 0
        gv_ = conv_gen(jobsV, [(xt[1], "xt1"), (hn[1], "hn1")], 1)
        gf0_ = front(0)
        dv_ = df_ = False
        while not (dv_ and df_):
            if not dv_:
                try:
                    next(gv_)
                except StopIteration:
                    dv_ = True
            if not df_:
                try:
                    next(gf0_)
                    nfront += 1
                except StopIteration:
                    df_ = True
        for nm_ in pr.dpools["m"][0]:
            if pr.dval[nm_] > 0:
                pr._wait("pool", (nm_, pr.dval[nm_]))
        for ti in range(ntiles):
            gb = back(ti)
            gf = front(ti + 1) if ti + 1 < ntiles else None
            done_b, done_f = False, gf is None
            step = 0
            fcount = 0
            while not (done_b and done_f):
                if not done_b:
                    try:
                        next(gb)
                    except StopIteration:
                        done_b = True
                step += 1
                target = nfront + 1 if done_b else (step * nfront) // FRONT_SPAN
                while not done_f and fcount < target:
                    try:
                        next(gf)
                        fcount += 1
                    except StopIteration:
                        done_f = True
                if done_b and not done_f:
                    continue

        for tok in out_toks:
            pr._wait("sp", tok)
        for e_ in ("act", "dve", "pe", "pool"):
            for tok in out_toks[-1:]:
                pr._wait(e_, tok)
    return nc


_NC_CACHE = {}


def _layout_inputs(inp, b):
    f = lambda a: np.ascontiguousarray(a, dtype=np.float32)
    m = {}
    m["x"] = f(inp["x"][b])
    m["c16"] = f(inp["c"][b].reshape(16, P))
    m["w_ada"] = f(inp["w_ada"][0])
    m["b_ada"] = f(inp["b_ada"][0].reshape(1, -1))
    m["g1row"] = f(inp["g_norm_mix"][0].reshape(1, -1))
    m["g2row"] = f(inp["g_norm_ffn"][0].reshape(1, -1))
    m["gf_b"] = f(np.broadcast_to(inp["g_final"].reshape(1, -1), (P, D)))
    m["gv_b"] = f(np.broadcast_to(inp["g_v"][0].reshape(1, -1), (P, 1024)))
    m["w_in"] = f(inp["w_in"][0])
    m["w_out"] = f(inp["w_out"][0])
    m["w_query"] = f(inp["w_query"][0])
    m["cw"] = f(inp["conv_w"][0].reshape(4, 8, P).transpose(2, 1, 0))
    m["cb"] = f(inp["conv_b"][0].reshape(8, P).T)
    m["wa"] = f(inp["w_gate_a"][0])
    m["wi"] = f(inp["w_gate_i"][0])
    m["ba"] = f(inp["b_gate_a"][0].T)
    m["bi"] = f(inp["b_gate_i"][0].T)
    m["lam"] = f(inp["lru_lambda"][0].reshape(8, P).T)
    m["wsT"] = f(inp["w_spatial"][0].transpose(2, 0, 1))
    m["bsrow"] = f(inp["b_spatial"][0].reshape(1, -1))
    m["keysT"] = f(inp["sub_keys"][0].reshape(16, P, P).transpose(2, 0, 1))
    m["expert_u"] = f(inp["expert_u"][0])
    m["expert_v"] = f(inp["expert_v"][0])
    return m


def kernel(**inputs):
    inp = {k: np.asarray(v) for k, v in inputs.items()}
    if "nc" not in _NC_CACHE:
        _NC_CACHE["nc"] = build_program()
    nc = _NC_CACHE["nc"]
    n = 8
    shared = _layout_inputs(inp, 0)
    in_maps = []
    for b in range(n):
        m = dict(shared)
        m["x"] = np.ascontiguousarray(inp["x"][b], dtype=np.float32)
        m["c16"] = np.ascontiguousarray(inp["c"][b].reshape(16, P), dtype=np.float32)
        in_maps.append(m)
    if DEBUG_CORES != 8:
        in_maps = in_maps[:DEBUG_CORES]
        n = DEBUG_CORES
    res = run_bass_kernel_spmd(nc, in_maps, core_ids=list(range(n)))
    out = np.stack([np.asarray(r["out"]) for r in res.results], axis=0)
    if DEBUG_TILES is not None:
        return out.astype(np.float32)
    return out.reshape(8, SEQ, D).astype(np.float32)
```

```python
import contextlib
import numpy as np
import concourse.bass as bass
import concourse.mybir as mybir
from concourse.bass_utils import run_bass_kernel_spmd

F32 = mybir.dt.float32
BF16 = mybir.dt.bfloat16
I32 = mybir.dt.int32
U32 = mybir.dt.uint32
AF = mybir.ActivationFunctionType
ALU = mybir.AluOpType

P = 128
D = 2048
SEQ = 2048
NT = SEQ // P
NDC = D // P
NEXP = 16384
EPS = 1e-6
GW = 256
NEG = -1.0e30
TAIL_SKEW = 3
FRONT_SPAN = 118

DEBUG_TILES = None
DEBUG_CORES = 8
DEBUG_NCH = 8
DEBUG_SLOT = True
DEBUG_STAGE = 0


class Prog:
    def __init__(self, nc, es):
        self.nc = nc
        self.eng = {"pe": nc.tensor, "act": nc.scalar, "dve": nc.vector, "pool": nc.gpsimd, "sp": nc.sync}
        self.semh = {}
        for k in self.eng:
            self.semh[k] = es.enter_context(nc.semaphore("sem_" + k))
        self.cnt = {k: 0 for k in self.eng}
        self.waited = {k: {} for k in self.eng}
        self.lastw = {}
        self.readers = {}
        self.dpools = {}
        self.dval = {}
        self.es = es

    def add_dma_pool(self, name, n):
        names = []
        for i in range(n):
            nm = "d_%s%d" % (name, i)
            self.semh[nm] = self.es.enter_context(self.nc.semaphore(nm))
            self.dval[nm] = 0
            names.append(nm)
        self.dpools[name] = [names, 0]

    def _wait(self, e, tok):
        nm, val = tok
        w = self.waited[e]
        if w.get(nm, 0) >= val:
            return
        self.eng[e].wait_ge(self.semh[nm], val)
        w[nm] = val

    def _deps(self, e, reads, writes):
        deps = set()
        for k in reads:
            t = self.lastw.get(k)
            if t is not None:
                deps.add(t)
        for k in writes:
            t = self.lastw.get(k)
            if t is not None:
                deps.add(t)
            for nm, val in self.readers.get(k, {}).items():
                deps.add((nm, val))
        for t in sorted(deps):
            if e == "pe" and t[0] == "pe":
                continue
            self._wait(e, t)

    def _record(self, tok, reads, writes):
        for k in writes:
            self.lastw[k] = tok
            self.readers[k] = {}
        for k in reads:
            r = self.readers.setdefault(k, {})
            if r.get(tok[0], 0) < tok[1]:
                r[tok[0]] = tok[1]

    def op(self, e, fn, reads=(), writes=()):
        self._deps(e, reads, writes)
        ins = fn(self.eng[e])
        self.cnt[e] += 1
        ins.then_inc(self.semh[e], 1)
        tok = (e, self.cnt[e])
        self._record(tok, reads, writes)
        return tok

    def dma(self, q, pool, fn, reads=(), writes=()):
        names, rr = self.dpools[pool]
        nm = names[rr % len(names)]
        self.dpools[pool][1] = rr + 1
        self._deps(q, reads, writes)
        if self.dval[nm] > 0:
            self._wait(q, (nm, self.dval[nm]))
        ins = fn(self.eng[q])
        self.dval[nm] += 16
        ins.then_inc(self.semh[nm], 16)
        tok = (nm, self.dval[nm])
        self._record(tok, reads, writes)
        return tok


def build_program():
    nc = bass.Bass("TRN2", target_bir_lowering=False)
    ntiles = NT if DEBUG_TILES is None else DEBUG_TILES

    def din(name, shape, dt=F32):
        return nc.dram_tensor(name, list(shape), dt, kind="ExternalInput").ap()

    x_d = din("x", [SEQ, D])
    c_d = din("c16", [16, P])
    wada_d = din("w_ada", [D, 6 * D])
    bada_d = din("b_ada", [1, 6 * D])
    g1_d = din("g1row", [1, D])
    g2_d = din("g2row", [1, D])
    gf_d = din("gf_b", [P, D])
    gv_d = din("gv_b", [P, 1024])
    win_d = din("w_in", [D, 4096])
    wout_d = din("w_out", [D, D])
    wq_d = din("w_query", [D, D])
    cw_d = din("cw", [P, 8, 4])
    cb_d = din("cb", [P, 8])
    wa_d = din("wa", [8, P, P])
    wi_d = din("wi", [8, P, P])
    ba_d = din("ba", [P, 8])
    bi_d = din("bi", [P, 8])
    lam_d = din("lam", [P, 8])
    wsT_d = din("wsT", [P, 8, P])
    bs_d = din("bsrow", [1, 1024])
    kT_d = din("keysT", [P, 16, P])
    eu_d = din("expert_u", [NEXP, D])
    ev_d = din("expert_v", [NEXP, D])
    out_d = nc.dram_tensor("out", [SEQ, D], F32, kind="ExternalOutput").ap()
    euv_d = nc.dram_tensor("euv_bf16", [NEXP, 2 * D], BF16, kind="Internal").ap()
    winb_d = nc.dram_tensor("win_bf16", [4096, D], BF16, kind="Internal").ap()
    woutb_d = nc.dram_tensor("wout_bf16", [D, D], BF16, kind="Internal").ap()
    wqb_d = nc.dram_tensor("wq_bf16", [D, D], BF16, kind="Internal").ap()

    wada_v = wada_d.rearrange("(kc k) n -> k kc n", k=P)
    win_v = win_d.rearrange("(kc k) n -> k kc n", k=P)
    wout_v = wout_d.rearrange("(kc k) n -> k kc n", k=P)
    win_rows = win_d.rearrange("r (h n) -> (r h) n", h=2)
    winb_v = winb_d.rearrange("(r h) n -> r (h n)", h=2).rearrange("(kc k) n -> k kc n", k=P)
    woutb_v = woutb_d.rearrange("(kc k) n -> k kc n", k=P)
    wq_v = wq_d.rearrange("(kc k) n -> k kc n", k=P)
    wqb_v = wqb_d.rearrange("(kc k) n -> k kc n", k=P)

    with contextlib.ExitStack() as es:
        es.enter_context(nc.allow_low_precision("bf16 expert-value accumulation with fp32 PSUM"))

        def sb(name, shape, dt=F32):
            return es.enter_context(nc.sbuf_tensor("s_" + name, list(shape), dt))

        def ps(name, shape, dt=F32):
            return es.enter_context(nc.psum_tensor("p_" + name, list(shape), dt))

        A2 = sb("A2", [P, D]); B2 = sb("B2", [P, D])
        GF = sb("GF", [P, D]); GV = sb("GV", [P, 1024])
        keysT = sb("keysT", [P, 16, P]); wa = sb("wa", [P, 8, P]); wi = sb("wi", [P, 8, P]); WmT = sb("WmT", [P, 8, P])
        ident = sb("ident", [P, P]); identb = sb("identb", [P, P], BF16)
        ones_row = sb("ones_row", [1, P]); bsrow = sb("bsrow", [1, 1024])
        cw = sb("cw", [P, 8, 4]); cb = sb("cb", [P, 8]); nba = sb("nba", [P, 8]); nbi = sb("nbi", [P, 8])
        lam = sb("lam", [P, 8]); cch = sb("cch", [P, 8]); cch2 = sb("cch2", [P, 8])
        A1col = sb("A1col", [P, 16]); B1col = sb("B1col", [P, 16]); scol = sb("scol", [P, 16]); c16 = sb("c16", [16, P])
        state = sb("state", [P, 8]); epsc = sb("epsc", [P, 1])
        wbuf = [sb("wbuf0", [P, NDC, GW]), sb("wbuf1", [P, NDC, GW])]
        xt = [sb("xt0", [P, D]), sb("xt1", [P, D])]; hn = [sb("hn0", [P, D]), sb("hn1", [P, D])]
        xl = sb("xl", [P, 8, P + 3]); ycat = sb("ycat", [P, NDC, P], BF16); hTb = sb("hTb", [P, NDC, P], BF16)
        tmpg = [sb("tmpg0", [P, P]), sb("tmpg1", [P, P])]
        vn = sb("vn", [P, 1024])
        NR = 2
        xc = [sb("xc%d" % i, [P, P]) for i in range(NR)]
        rt = [sb("rt%d" % i, [P, P]) for i in range(NR)]
        it = [sb("it%d" % i, [P, P]) for i in range(NR)]
        at = [sb("at%d" % i, [P, P]) for i in range(NR)]
        qTs = [sb("qTs0", [P, P]), tmpg[1]]
        stt = sb("stt", [P, 16]); sttb = sb("sttb", [P, 16])
        v12 = sb("v12", [P, 2, 16]); iu = sb("iu", [P, 2, 16], U32); if12 = sb("if12", [P, 2, 16])
        tmpk = tmpg[0]; cand = sb("cand", [P, 16, 16]); cand2 = sb("cand2", [P, 256]); cidx = sb("cidx", [P, 16, 16])
        j256 = cand2; tv = sb("tv", [P, 16]); ew = sb("ew", [P, 16]); tks = sb("tks", [P, 4])
        idxf = sb("idxf", [P, P]); idxi = [sb("idxi0", [P, P], I32), sb("idxi1", [P, P], I32)]; gw = [sb("gw0", [P, P]), sb("gw1", [P, P])]
        zt = sb("zt", [P, P]); actt = sb("actt", [P, P])
        NCB = 7
        CB = [sb("CB%d" % i, [P, 2 * D], BF16) for i in range(NCB)]
        GTM = CB[4][:, :].bitcast(F32)
        GTF = CB[5][:, :].bitcast(F32)
        kGTM = ["modtile2", ("CBu", 4), ("CBv", 4)]
        kGTF = ["modtile5", ("CBu", 5), ("CBv", 5)]
        Hb = sb("hb", [P, D], BF16)
        Dg = [sb("Dg0", [P, P], BF16), sb("Dg1", [P, P], BF16)]
        big = ps("big", [P, D])
        yacc = ps("yacc", [P, D])

        pr = Prog(nc, es)
        pr.add_dma_pool("w", 4)
        pr.add_dma_pool("m", 6)
        pr.add_dma_pool("g", 8)

        BIG = [("big", j) for j in range(4)]
        HT = [("hT", j) for j in range(NDC)]
        YC = [("ycat", j) for j in range(NDC)]
        XL = [("xl", j) for j in range(8)]

        wseq = []
        for g in range(6 * D // GW):
            wseq.append((wada_v, g, False))
        per_tile = [(winb_v, g, True) for g in (0, 1, 2, 3, 6, 7, 4, 5)] + [(woutb_v, g, True) for g in range(4)] + \
                   [(wqb_v, g, True) for g in range(4)]
        for _ in range(ntiles):
            wseq.extend(per_tile)
        wstate = {"issued": 0, "used": 0}

        def w_issue_upto(n):
            while wstate["issued"] <= n and wstate["issued"] < len(wseq):
                i = wstate["issued"]
                view, g, isb = wseq[i]
                slot = i % 2
                if isb:
                    pr.dma("sp", "w", lambda e, view=view, g=g, slot=slot: e.dma_start(
                        out=wbuf[slot][:, :, :].bitcast(BF16), in_=view[:, :, g * 512:(g + 1) * 512]), reads=(), writes=[("w", slot)])
                else:
                    pr.dma("sp", "w", lambda e, view=view, g=g, slot=slot: e.dma_start(
                        out=wbuf[slot][:, :, :], in_=view[:, :, g * GW:(g + 1) * GW]), reads=(), writes=[("w", slot)])
                wstate["issued"] += 1

        def w_next():
            i = wstate["used"]
            w_issue_upto(i)
            wstate["used"] += 1
            wb_ = wbuf[i % 2][:, :, :].bitcast(BF16) if wseq[i][2] else wbuf[i % 2]
            return wb_, ("w", i % 2), i

        def w_prefetch():
            w_issue_upto(wstate["used"])

        def mload(dst, src, key):
            pr.dma("sp", "m", lambda e: e.dma_start(out=dst, in_=src), reads=(), writes=[key])

        mload(cw[:, :, :], cw_d[:, :, :], "cw"); mload(cb[:, :], cb_d[:, :], "cb")
        mload(nba[:, :], ba_d[:, :], "nba"); mload(nbi[:, :], bi_d[:, :], "nbi"); mload(lam[:, :], lam_d[:, :], "lam")
        mload(wa[:, :, :], wa_d.rearrange("h i j -> i h j"), "wa"); mload(wi[:, :, :], wi_d.rearrange("h i j -> i h j"), "wi")
        mload(WmT[:, :, :], wsT_d[:, :, :], "WmT")
        mload(bsrow[:, :], bs_d[:, :], "bsrow"); mload(c16[:, :], c_d[:, :], "c16")
        mload(GF[:, :], gf_d[:, :], "GF"); mload(GV[:, :], gv_d[:, :], "GV")
        w_prefetch()

        pr.op("pool", lambda e: e.memset(ident[:, :], 1.0), writes=["ident"])
        pr.op("pool", lambda e: e.affine_select(out=ident[:, :], in_=ident[:, :], pattern=[[1, P]], compare_op=ALU.is_equal,
                                                fill=0.0, base=0, channel_multiplier=-1), reads=["ident"], writes=["ident"])
        pr.op("pool", lambda e: e.tensor_copy(out=identb[:, :], in_=ident[:, :]), reads=["ident"], writes=["identb"])
        pr.op("pool", lambda e: e.memset(ones_row[:, :], 1.0), writes=["ones_row"])
        pr.op("pool", lambda e: e.memset(state[:, :], 0.0), writes=["state"])
        pr.op("pool", lambda e: e.memset(epsc[:, :], EPS), writes=["epsc"])
        pr.op("pool", lambda e: e.memset(xl[:, :, :], 0.0), writes=XL + ["xlh"])
        pr.op("pool", lambda e: e.affine_select(out=WmT[:, :, :], in_=WmT[:, :, :], pattern=[[0, 8], [1, P]], compare_op=ALU.is_ge,
                                                fill=0.0, base=0, channel_multiplier=-1), reads=["WmT"], writes=["WmT"])
        pr.op("dve", lambda e: e.tensor_scalar(out=nba[:, :], in0=nba[:, :], scalar1=-1.0, scalar2=None, op0=ALU.mult), reads=["nba"], writes=["nba"])
        pr.op("dve", lambda e: e.tensor_scalar(out=nbi[:, :], in0=nbi[:, :], scalar1=-1.0, scalar2=None, op0=ALU.mult), reads=["nbi"], writes=["nbi"])
        pr.op("act", lambda e: e.activation(out=cch[:, :], in_=lam[:, :], func=AF.Exp, scale=-1.0), reads=["lam"], writes=["cch"])
        pr.op("act", lambda e: e.activation(out=cch[:, :], in_=cch[:, :], func=AF.Ln, bias=1.0, scale=1.0), reads=["cch"], writes=["cch"])
        pr.op("dve", lambda e: e.tensor_scalar(out=cch2[:, :], in0=cch[:, :], scalar1=-16.0, scalar2=None, op0=ALU.mult), reads=["cch"], writes=["cch2"])
        pr.op("dve", lambda e: e.tensor_scalar(out=cch[:, :], in0=cch[:, :], scalar1=-8.0, scalar2=None, op0=ALU.mult), reads=["cch", "cch2"], writes=["cch"])
        pr.op("pe", lambda e: e.transpose(out=big[:, 512:528], in_=c16[:, :], identity=ident[0:16, 0:16]), reads=["c16", "ident"], writes=[("big", 1)])
        pr.op("act", lambda e: e.activation(out=scol[:, :], in_=big[:, 512:528], func=AF.Silu), reads=[("big", 1)], writes=["scol"])

        stage = [(xt[0], "xt0"), (xt[1], "xt1"), (hn[0], "hn0"), (hn[1], "hn1")]
        jobs = [(win_rows, winb_d, i, None) for i in range(4096 // P)] + [(wq_d, wqb_d, i, None) for i in range(D // P)] + \
               [(eu_d, euv_d[:, 0:D], i, None) for i in range(NEXP // P)]
        NPLAIN = len(jobs)
        jobs += [(wout_d, woutb_d, i, "m") for i in range(D // P)]
        jobsV = [(ev_d, euv_d[:, D:2 * D], i, "f") for i in range(NEXP // P)]

        def conv_gen(jobs, stage, LOOK):
            NS = len(stage)
            for i in range(len(jobs) + LOOK):
                if i < len(jobs):
                    src, _, r, _ = jobs[i]
                    st_, sk_ = stage[i % NS]
                    pr.dma("sp", "m", lambda e: e.dma_start(out=st_[:, :], in_=src[r * P:(r + 1) * P, :]), writes=[sk_])
                j = i - LOOK
                if j >= 0:
                    _, dst, r, sc_ = jobs[j]
                    st_, sk_ = stage[j % NS]
                    gb_, gk_ = CB[j % 4][:, 0:D], ("CBu", j % 4)
                    ce = ("act", "dve", "pool")[j % 3]
                    if sc_ is not None:
                        gt_, kgt_ = (GTM, kGTM) if sc_ == "m" else (GTF, kGTF)
                        pr.op("dve", lambda e: e.scalar_tensor_tensor(out=gb_[:, :], in0=st_[:, :], scalar=1.0, op0=ALU.mult, in1=gt_[:, :], op1=ALU.mult),
                              reads=[sk_] + kgt_, writes=[gk_])
                    elif ce == "act":
                        pr.op("act", lambda e: e.activation(out=gb_[:, :], in_=st_[:, :], func=AF.Copy), reads=[sk_], writes=[gk_])
                    else:
                        pr.op(ce, lambda e: e.tensor_copy(out=gb_[:, :], in_=st_[:, :]), reads=[sk_], writes=[gk_])
                    pr.dma("sp", "m", lambda e: e.dma_start(out=dst[r * P:(r + 1) * P, :], in_=gb_[:, :]), reads=[gk_], writes=[])
                yield i

        rowt = GTF[0:1, :]
        g1row = B2[0:1, :]
        g2row = A2[0:1, :]
        badarow = keysT[0:1, :, :].rearrange("p a b -> p (a b)")
        kROW, kG1, kG2, kBA = "modtile5", "modtile3", "modtile4", "keysT"

        def adaln_gen():
            mload(g1row, g1_d[:, :], kG1)
            mload(g2row, g2_d[:, :], kG2)
            for v in range(6):
                pr.dma("sp", "m", lambda e, v=v: e.dma_start(out=badarow, in_=bada_d[:, v * D:(v + 1) * D]), writes=[kBA])
                for g in range(D // GW):
                    wb, wkey, _ = w_next()
                    if wstate["used"] < 6 * D // GW:
                        w_prefetch()
                    for kc in range(NDC):
                        pr.op("pe", lambda e, kc=kc, g=g, wb=wb: e.matmul(big[0:1, g * GW:(g + 1) * GW], lhsT=scol[:, kc:kc + 1], rhs=wb[:, kc, :],
                                                                        start=(kc == 0), stop=(kc == NDC - 1)),
                              reads=["scol", wkey], writes=[("big", g // 2)])
                    yield
                pr.op("dve", lambda e: e.tensor_tensor(out=rowt, in0=big[0:1, :], in1=badarow, op=ALU.add), reads=BIG + [kBA], writes=[kROW])
                if v == 1:
                    pr.op("dve", lambda e: e.scalar_tensor_tensor(out=rowt, in0=rowt, scalar=1.0, op0=ALU.add, in1=g1row, op1=ALU.mult),
                          reads=[kROW, kG1], writes=[kROW])
                if v == 4:
                    pr.op("dve", lambda e: e.scalar_tensor_tensor(out=rowt, in0=rowt, scalar=1.0, op0=ALU.add, in1=g2row, op1=ALU.mult),
                          reads=[kROW, kG2], writes=[kROW])
                if v in (0, 1):
                    for dc in range(NDC):
                        pr.op("pe", lambda e, dc=dc: e.matmul(big[:, 1024 + 2 * dc:1024 + 2 * dc + 2], lhsT=rowt[0:1, dc * P:(dc + 1) * P], rhs=ones_row[0:1, 0:2],
                                                            start=True, stop=True), reads=[kROW, "ones_row"], writes=[("big", 2)])
                    dst = B1col if v == 0 else A1col
                    pr.op("act", lambda e, dst=dst: e.activation(out=dst[:, :], in_=big[:, 1024:1056].rearrange("p (a b) -> p a b", b=2)[:, :, 0], func=AF.Copy),
                          reads=[("big", 2)], writes=["B1col" if v == 0 else "A1col"])
                else:
                    dst = {2: GTM, 3: B2, 4: A2, 5: GTF}[v]
                    for j in range(4):
                        pr.op("pe", lambda e, j=j: e.matmul(big[:, j * 512:(j + 1) * 512], lhsT=ones_row[0:1, :], rhs=rowt[0:1, j * 512:(j + 1) * 512],
                                                          start=True, stop=True), reads=[kROW, "ones_row"], writes=[("big", j)])
                    pr.op("act", lambda e, dst=dst: e.activation(out=dst[:, :], in_=big[:, :], func=AF.Copy), reads=BIG,
                          writes=(kGTM if v == 2 else kGTF if v == 5 else ["modtile%d" % v]))
                yield

        gc_, ga_ = conv_gen(jobs, stage, 3), adaln_gen()
        dc_ = da_ = False
        it_ = 0
        while not (dc_ and da_):
            it_ += 1
            if not dc_:
                try:
                    ji_ = next(gc_)
                    if ji_ + 1 >= NPLAIN:
                        for _ in ga_:
                            pass
                        da_ = True
                except StopIteration:
                    dc_ = True
            if not da_ and (dc_ or it_ % 3 == 0):
                try:
                    next(ga_)
                except StopIteration:
                    da_ = True
        for nm_ in pr.dpools["m"][0]:
            if pr.dval[nm_] > 0:
                pr._wait("sp", (nm_, pr.dval[nm_]))
        mload(keysT[:, :, :], kT_d[:, :, :], "keysT")
        w_prefetch()

        MT = ["modtile3", "modtile4"]

        def rstd_from_ssq(ssq_col, out_col, n, key_in, key_out):
            pr.op("act", lambda e: e.activation(out=out_col, in_=ssq_col, func=AF.Ln, scale=1.0 / n, bias=epsc[:, 0:1]), reads=[key_in, "epsc"], writes=[key_out])
            pr.op("act", lambda e: e.activation(out=out_col, in_=out_col, func=AF.Exp, scale=-0.5), reads=[key_out], writes=[key_out])

        out_toks = []

        def front(ti):
            p = ti % 2
            X, H = xt[p], hn[p]
            kX, kH, kI, kG = "xt%d" % p, "hn%d" % p, "idxi%d" % p, "gw%d" % p
            r0 = ti * P
            pr.dma("sp", "m", lambda e: e.dma_start(out=X[:, :], in_=x_d[r0:r0 + P, :]), writes=[kX])
            yield
            pr.op("act", lambda e: e.activation(out=ycat[:, :, :].rearrange("p a b -> p (a b)"), in_=X[:, :], func=AF.Square, accum_out=stt[:, 0:1]),
                  reads=[kX], writes=YC + ["ssq1"])
            rstd_from_ssq(stt[:, 0:1], stt[:, 1:2], D, "ssq1", "rstd1")
            pr.op("act", lambda e: e.activation(out=H[:, :], in_=X[:, :], func=AF.Copy, scale=stt[:, 1:2]), reads=[kX, "rstd1"], writes=[kH])
            yield
            for dc in range(NDC):
                pr.op("pe", lambda e, dc=dc: e.transpose(out=big[:, dc * P:(dc + 1) * P], in_=H[:, dc * P:(dc + 1) * P], identity=ident[:, :]),
                      reads=[kH, "ident"], writes=[("big", dc // 4)])
            yield
            for dc in range(NDC):
                pr.op("act", lambda e, dc=dc: e.activation(out=hTb[:, dc, :], in_=big[:, dc * P:(dc + 1) * P], func=AF.Identity,
                                                          scale=A1col[:, dc:dc + 1], bias=B1col[:, dc:dc + 1]),
                      reads=[("big", dc // 4), "A1col", "B1col"], writes=[("hTb", dc)])
            yield
            if ti > 0:
                pr.op("dve", lambda e: e.tensor_copy(out=xl[:, :, 0:3], in_=xl[:, :, P:P + 3]), reads=XL, writes=["xlh"])

            cur = {}

            def fm_chunk(c24, bank, evac):
                if c24 % 4 == 0:
                    cur["w"] = w_next()
                    w_prefetch()
                wb, wkey, _ = cur["w"]
                for dc in range(NDC):
                    pr.op("pe", lambda e, dc=dc: e.matmul(big[:, bank * 512:bank * 512 + P], lhsT=wb[:, dc, (c24 % 4) * P:(c24 % 4 + 1) * P], rhs=hTb[:, dc, :],
                                                        start=(dc == 0), stop=(dc == NDC - 1)),
                          reads=[wkey, ("hTb", dc)], writes=[("big", bank)])
                evac(big[:, bank * 512:bank * 512 + P], ("big", bank))

            for c in range(8):
                fm_chunk(c, c % 2, lambda src, key, c=c: pr.op("act", lambda e: e.activation(out=xl[:, c, 3:3 + P], in_=src, func=AF.Copy),
                                                                reads=[key], writes=[("xl", c)]))
                yield

            def lru_chunk(c):
                k = c % NR
                Xc, R, I, A, A2t, Hs = xc[k], rt[k], it[k], at[k], rt[k], xc[k]
                kx, kr, ki, ka, ka2, kh = ("xc", k), ("rt", k), ("it", k), ("at", k), ("rt", k), ("xc", k)
                pr.op("dve", lambda e: e.tensor_scalar(out=Xc[:, :], in0=xl[:, c, 3:3 + P], scalar1=cw[:, c, 3:4], scalar2=cb[:, c:c + 1],
                                                       op0=ALU.mult, op1=ALU.add), reads=[("xl", c), "cw", "cb"], writes=[kx])
                for kk in (2, 1, 0):
                    pr.op("dve", lambda e, kk=kk: e.scalar_tensor_tensor(out=Xc[:, :], in0=xl[:, c, kk:kk + P], scalar=cw[:, c, kk:kk + 1], op0=ALU.mult,
                                                                          in1=Xc[:, :], op1=ALU.add), reads=[("xl", c), "xlh", "cw", kx], writes=[kx])
                yield
                gbk = 2 + c % 2
                gk_ = ("big", gbk)
                gR = big[:, gbk * 512:gbk * 512 + P]
                gI = big[:, gbk * 512 + P:gbk * 512 + 2 * P]
                pr.op("pe", lambda e: e.matmul(gR, lhsT=wa[:, c, :], rhs=Xc[:, :], start=True, stop=True), reads=["wa", kx], writes=[gk_])
                pr.op("pe", lambda e: e.matmul(gI, lhsT=wi[:, c, :], rhs=Xc[:, :], start=True, stop=True), reads=["wi", kx], writes=[gk_])
                yield
                pr.op("act", lambda e: e.activation(out=R[:, :], in_=gR, func=AF.Exp, scale=-1.0, bias=nba[:, c:c + 1]), reads=[gk_, "nba"], writes=[kr])
                pr.op("act", lambda e: e.activation(out=I[:, :], in_=gI, func=AF.Exp, scale=-1.0, bias=nbi[:, c:c + 1]), reads=[gk_, "nbi"], writes=[ki])
                yield
                pr.op("dve", lambda e: e.tensor_scalar(out=R[:, :], in0=R[:, :], scalar1=1.0, scalar2=None, op0=ALU.add), reads=[kr], writes=[kr])
                pr.op("dve", lambda e: e.reciprocal(out=R[:, :], in_=R[:, :]), reads=[kr], writes=[kr])
                pr.op("dve", lambda e: e.tensor_scalar(out=I[:, :], in0=I[:, :], scalar1=1.0, scalar2=None, op0=ALU.add), reads=[ki], writes=[ki])
                pr.op("dve", lambda e: e.reciprocal(out=I[:, :], in_=I[:, :]), reads=[ki], writes=[ki])
                pr.op("dve", lambda e: e.tensor_tensor(out=I[:, :], in0=I[:, :], in1=Xc[:, :], op=ALU.mult), reads=[ki, kx], writes=[ki])
                yield
                pr.op("act", lambda e: e.activation(out=A[:, :], in_=R[:, :], func=AF.Exp, scale=cch[:, c:c + 1]), reads=[kr, "cch"], writes=[ka])
                pr.op("act", lambda e: e.activation(out=A2t[:, :], in_=R[:, :], func=AF.Exp, scale=cch2[:, c:c + 1]), reads=[kr, "cch2"], writes=[ka2])
                yield
                pr.op("dve", lambda e: e.tensor_scalar(out=A2t[:, :], in0=A2t[:, :], scalar1=-1.0, scalar2=1.0, op0=ALU.mult, op1=ALU.add),
                      reads=[ka2], writes=[ka2])
                pr.op("dve", lambda e: e.tensor_scalar(out=A2t[:, :], in0=A2t[:, :], scalar1=1e-30, scalar2=None, op0=ALU.max), reads=[ka2], writes=[ka2])
                yield
                pr.op("act", lambda e: e.activation(out=A2t[:, :], in_=A2t[:, :], func=AF.Ln), reads=[ka2], writes=[ka2])
                pr.op("act", lambda e: e.activation(out=A2t[:, :], in_=A2t[:, :], func=AF.Exp, scale=0.5), reads=[ka2], writes=[ka2])
                yield
                pr.op("dve", lambda e: e.tensor_tensor(out=I[:, :], in0=I[:, :], in1=A2t[:, :], op=ALU.mult), reads=[ki, ka2], writes=[ki])
                pr.op("dve", lambda e: e.tensor_tensor_scan(out=Hs[:, :], data0=A[:, :], data1=I[:, :], initial=state[:, c:c + 1],
                                                            op0=ALU.mult, op1=ALU.add), reads=[ka, ki, "state"], writes=[kh])
                pr.op("dve", lambda e: e.tensor_copy(out=state[:, c:c + 1], in_=Hs[:, P - 1:P]), reads=[kh], writes=["state"])
                pr.op("dve", lambda e: e.tensor_copy(out=ycat[:, c, :], in_=Hs[:, :]), reads=[kh], writes=[("ycat", c)])
                yield

            for c0 in range(0, 8, 2):
                ga, gb2 = lru_chunk(c0), lru_chunk(c0 + 1)
                for _ in zip(ga, gb2):
                    yield
            pend = None

            def y_mul(c):
                tg, ktg = tmpg[c % 2], ("tmpg", c % 2)
                pr.op("dve", lambda e: e.tensor_tensor(out=ycat[:, c, :], in0=ycat[:, c, :], in1=tg[:, :], op=ALU.mult),
                      reads=[("ycat", c), ktg], writes=[("ycat", c)])

            for c in range(8):
                tg, ktg = tmpg[c % 2], ("tmpg", c % 2)
                if pend is not None and c >= 2:
                    pass
                fm_chunk(8 + c, c % 2, lambda src, key: pr.op("act", lambda e: e.activation(out=tg[:, :], in_=src, func=AF.Gelu),
                                                               reads=[key], writes=[ktg]))
                yield
                y_mul(c)
            yield
            for g2 in range(2):
                wb, wkey, _ = w_next()
                w_prefetch()
                for dc in range(NDC):
                    pr.op("pe", lambda e, dc=dc: e.matmul(big[:, g2 * 512:(g2 + 1) * 512], lhsT=hTb[:, dc, :], rhs=wb[:, dc, :],
                                                        start=(dc == 0), stop=(dc == NDC - 1)),
                          reads=[wkey, ("hTb", dc)], writes=[("big", g2)])
                yield
            pr.op("act", lambda e: e.activation(out=vn[:, :], in_=big[:, 0:1024], func=AF.Gelu), reads=[("big", 0), ("big", 1)], writes=["vn"])
            pr.op("act", lambda e: e.activation(out=ycat[:, 8:16, :].rearrange("p a b -> p (a b)"), in_=vn[:, :], func=AF.Square, accum_out=stt[:, 2:3]),
                  reads=["vn"], writes=[("ycat", j) for j in range(8, 16)] + ["ssqv"])
            rstd_from_ssq(stt[:, 2:3], stt[:, 3:4], 1024, "ssqv", "rstdv")
            yield
            pr.op("dve", lambda e: e.scalar_tensor_tensor(out=vn[:, :], in0=vn[:, :], scalar=stt[:, 3:4], op0=ALU.mult, in1=GV[:, :], op1=ALU.mult),
                  reads=["vn", "rstdv", "GV"], writes=["vn"])
            yield

            def sgu_mul(g):
                tg, ktg = tmpg[g % 2], ("tmpg", g % 2)
                sbk = 2 + g % 2
                pr.op("dve", lambda e: e.tensor_tensor(out=ycat[:, 8 + g, :], in0=tg[:, :], in1=big[:, sbk * 512:sbk * 512 + P], op=ALU.mult),
                      reads=[ktg, ("big", sbk)], writes=[("ycat", 8 + g)])

            for g in range(8):
                tg, ktg = tmpg[g % 2], ("tmpg", g % 2)
                fm_chunk(16 + g, g % 2, lambda src, key: pr.op("act", lambda e: e.activation(out=tg[:, :], in_=src, func=AF.Gelu),
                                                                reads=[key], writes=[ktg]))
                sbk = 2 + g % 2
                sk_ = ("big", sbk)
                sT = big[:, sbk * 512:sbk * 512 + P]
                pr.op("pe", lambda e: e.matmul(sT, lhsT=vn[:, g * P:(g + 1) * P], rhs=WmT[:, g, :], start=True, stop=False),
                      reads=["vn", "WmT"], writes=[sk_])
                pr.op("pe", lambda e: e.matmul(sT, lhsT=ones_row[0:1, :], rhs=bsrow[0:1, g * P:(g + 1) * P], start=False, stop=True),
                      reads=["ones_row", "bsrow"], writes=[sk_])
                yield
                sgu_mul(g)
            yield
            for g in range(4):
                wb, wkey, _ = w_next()
                w_prefetch()
                for fc in range(NDC):
                    pr.op("pe", lambda e, fc=fc: e.matmul(big[:, g * 512:(g + 1) * 512], lhsT=ycat[:, fc, :], rhs=wb[:, fc, :],
                                                        start=(fc == 0), stop=(fc == NDC - 1)),
                          reads=[wkey, ("ycat", fc)], writes=[("big", g)])
                yield
            pr.op("dve", lambda e: e.tensor_tensor(out=X[:, :], in0=X[:, :], in1=big[:, :], op=ALU.add), reads=[kX] + BIG, writes=[kX])
            yield
            pr.op("act", lambda e: e.activation(out=ycat[:, :, :].rearrange("p a b -> p (a b)"), in_=X[:, :], func=AF.Square, accum_out=stt[:, 4:5]),
                  reads=[kX], writes=YC + ["ssq2"])
            rstd_from_ssq(stt[:, 4:5], stt[:, 5:6], D, "ssq2", "rstd2")
            yield
            pr.op("dve", lambda e: e.scalar_tensor_tensor(out=H[:, :], in0=X[:, :], scalar=stt[:, 5:6], op0=ALU.mult, in1=A2[:, :], op1=ALU.mult),
                  reads=[kX, "rstd2"] + MT, writes=[kH])
            pr.op("dve", lambda e: e.tensor_tensor(out=H[:, :], in0=H[:, :], in1=B2[:, :], op=ALU.add), reads=[kH] + MT, writes=[kH])
            yield
            for dc in range(NDC):
                pr.op("pe", lambda e, dc=dc: e.transpose(out=big[:, dc * P:(dc + 1) * P], in_=H[:, dc * P:(dc + 1) * P], identity=ident[:, :]),
                      reads=[kH, "ident"], writes=[("big", dc // 4)])
            yield
            for j in range(4):
                pr.op("act", lambda e, j=j: e.activation(out=hTb[:, 4 * j:4 * j + 4, :], in_=big[:, j * 512:(j + 1) * 512].rearrange("p (a b) -> p a b", b=P),
                                                        func=AF.Copy), reads=[("big", j)], writes=[("hTb", 4 * j + i) for i in range(4)])
            yield

            def head_scores(h):
                if h % 2 == 0:
                    cur["wq"] = w_next()
                    w_prefetch()
                wb, wkey, _ = cur["wq"]
                sbk = 2 + h % 2
                kb = ("big", sbk)
                for p_ in range(2):
                    hp = 2 * h + p_
                    qb_ = big[:, p_ * 512:p_ * 512 + P]
                    qk_ = ("big", p_)
                    for dc in range(NDC):
                        co = ((h % 2) * 2 + p_) * P
                        pr.op("pe", lambda e, dc=dc: e.matmul(qb_, lhsT=wb[:, dc, co:co + P], rhs=hTb[:, dc, :],
                                                            start=(dc == 0), stop=(dc == NDC - 1)),
                              reads=[wkey, ("hTb", dc)], writes=[qk_])
                    q = qTs[p_]
                    pr.op("act", lambda e: e.activation(out=q[:, :], in_=qb_, func=AF.Copy), reads=[qk_], writes=[(("qTs", 0) if p_ == 0 else ("tmpg", 1))])
                    yield
                    pr.op("pe", lambda e: e.matmul(big[:, sbk * 512 + p_ * P:sbk * 512 + (p_ + 1) * P], lhsT=q[:, :], rhs=keysT[:, hp, :], start=True, stop=True),
                          reads=[(("qTs", 0) if p_ == 0 else ("tmpg", 1)), "keysT"], writes=[kb])

            def head_topk(h):
                sbk = 2 + h % 2
                kb = ("big", sbk)
                for p_ in range(2):
                    sP = big[:, sbk * 512 + p_ * P:sbk * 512 + (p_ + 1) * P]
                    pr.op("dve", lambda e: e.max(out=v12[:, p_, 0:8], in_=sP), reads=[kb], writes=["v12"])
                    pr.op("dve", lambda e: e.max_index(out=iu[:, p_, 0:8], in_max=v12[:, p_, 0:8], in_values=sP), reads=[kb, "v12"], writes=["iu"])
                    pr.op("dve", lambda e: e.match_replace(out=tmpk[:, :], in_to_replace=v12[:, p_, 0:8], in_values=sP, imm_value=NEG),
                          reads=[kb, "v12"], writes=[("tmpg", 0)])
                    pr.op("dve", lambda e: e.max(out=v12[:, p_, 8:16], in_=tmpk[:, :]), reads=[("tmpg", 0)], writes=["v12"])
                    pr.op("dve", lambda e: e.max_index(out=iu[:, p_, 8:16], in_max=v12[:, p_, 8:16], in_values=tmpk[:, :]), reads=[("tmpg", 0), "v12"], writes=["iu"])
                    yield
                pr.op("dve", lambda e: e.tensor_copy(out=if12[:, :, :], in_=iu[:, :, :]), reads=["iu"], writes=["if12"])
                pr.op("dve", lambda e: e.tensor_tensor(out=cand[:, :, :], in0=v12[:, 0, :, None].broadcast_to([P, 16, 16]),
                                                       in1=v12[:, 1, None, :].broadcast_to([P, 16, 16]), op=ALU.add), reads=["v12"], writes=["cand"])
                pr.op("dve", lambda e: e.scalar_tensor_tensor(out=cidx[:, :, :], in0=if12[:, 0, :, None].broadcast_to([P, 16, 16]), scalar=float(P), op0=ALU.mult,
                                                              in1=if12[:, 1, None, :].broadcast_to([P, 16, 16]), op1=ALU.add), reads=["if12"], writes=["cidx"])
                candf = cand[:, :, :].rearrange("p a b -> p (a b)")
                cidxf = cidx[:, :, :].rearrange("p a b -> p (a b)")
                pr.op("dve", lambda e: e.max(out=tv[:, 0:8], in_=candf), reads=["cand"], writes=["tv"])
                pr.op("dve", lambda e: e.match_replace(out=cand2[:, :], in_to_replace=tv[:, 0:8], in_values=candf, imm_value=NEG),
                      reads=["cand", "tv"], writes=["cand2"])
                pr.op("dve", lambda e: e.max(out=tv[:, 8:16], in_=cand2[:, :]), reads=["cand2"], writes=["tv"])
                pr.op("dve", lambda e: e.tensor_scalar(out=tks[:, 0:1], in0=tv[:, 0:1], scalar1=-1.0, scalar2=None, op0=ALU.mult), reads=["tv"], writes=["tks0"])
                pr.op("act", lambda e: e.activation(out=ew[:, :], in_=tv[:, :], func=AF.Exp, bias=tks[:, 0:1], scale=1.0, accum_out=tks[:, 1:2]),
                      reads=["tv", "tks0"], writes=["ew", "tks1"])
                yield
                for k in range(16):
                    pr.op("dve", lambda e, k=k: e.scalar_tensor_tensor(out=j256[:, :], in0=candf, scalar=tv[:, k:k + 1], op0=ALU.is_equal, in1=cidxf, op1=ALU.mult,
                                                                        accum_out=idxf[:, h * 16 + k:h * 16 + k + 1]),
                          reads=["cand", "cidx", "tv"], writes=["cand2", ("idxf", h * 16 + k)])
                    if k % 4 == 3:
                        yield
                pr.op("dve", lambda e: e.reciprocal(out=tks[:, 2:3], in_=tks[:, 1:2]), reads=["tks1"], writes=["tks2"])
                pr.op("dve", lambda e: e.tensor_scalar(out=gw[p][:, h * 16:(h + 1) * 16], in0=ew[:, :], scalar1=tks[:, 2:3], scalar2=None, op0=ALU.mult),
                      reads=["ew", "tks2"], writes=[kG])

            for _ in head_scores(0):
                yield
            for h in range(8):
                gs_ = head_scores(h + 1) if h + 1 < 8 else iter(())
                gt_ = head_topk(h)
                done_s = done_t = False
                while not (done_s and done_t):
                    if not done_s:
                        try:
                            next(gs_)
                        except StopIteration:
                            done_s = True
                    if not done_t:
                        try:
                            next(gt_)
                        except StopIteration:
                            done_t = True
                    yield
            IDXF = [("idxf", i) for i in range(P)]
            pr.op("dve", lambda e: e.tensor_scalar(out=idxf[:, :], in0=idxf[:, :], scalar1=float(NEXP - 1), scalar2=0.0, op0=ALU.min, op1=ALU.max),
                  reads=IDXF, writes=IDXF)
            pr.op("dve", lambda e: e.tensor_copy(out=idxi[p][:, :], in_=idxf[:, :]), reads=IDXF, writes=[kI])
            yield

        gcnt = [0]

        def back(ti):
            p = ti % 2
            X, H = xt[p], hn[p]
            kX, kH, kI, kG = "xt%d" % p, "hn%d" % p, "idxi%d" % p, "gw%d" % p
            r0 = ti * P
            pr.op("act", lambda e: e.activation(out=Hb[:, :], in_=H[:, :], func=AF.Copy), reads=[kH], writes=["hb"])
            yield

            def head(s_, b):
                pr.dma("pool", "g", lambda e: e.indirect_dma_start(out=CB[b][:, :], out_offset=None, in_=euv_d[:, :],
                                                                   in_offset=bass.IndirectOffsetOnAxis(ap=idxi[p][:, s_:s_ + 1], axis=0)),
                       reads=[kI], writes=[("CBu", b), ("CBv", b)])
                pr.op("dve", lambda e: e.tensor_tensor(out=CB[b][:, 0:D], in0=CB[b][:, 0:D], in1=Hb[:, :], op=ALU.mult),
                      reads=[("CBu", b), "hb"], writes=[("CBu", b)])
                pr.op("act", lambda e: e.activation(out=CB[b][:, 0:D], in_=CB[b][:, 0:D], func=AF.Copy, accum_out=zt[:, s_:s_ + 1]),
                      reads=[("CBu", b)], writes=[("CBu", b), ("zt", s_)])
                pr.op("act", lambda e: e.activation(out=actt[:, s_:s_ + 1], in_=zt[:, s_:s_ + 1], func=AF.Gelu),
                      reads=[("zt", s_)], writes=[("actt", s_)])

            def tail(s_, b):
                d2 = s_ % 2
                pr.op("dve", lambda e: e.tensor_scalar(out=Dg[d2][:, :], in0=identb[:, :], scalar1=actt[:, s_:s_ + 1], scalar2=gw[p][:, s_:s_ + 1],
                                                       op0=ALU.mult, op1=ALU.mult),
                      reads=["identb", ("actt", s_), kG], writes=[("Dg", d2)])
                for j in range(4):
                    pr.op("pe", lambda e, j=j: e.matmul(yacc[:, j * 512:(j + 1) * 512], lhsT=Dg[d2][:, :], rhs=CB[b][:, D + j * 512:D + (j + 1) * 512],
                                                      start=(s_ == 0), stop=(s_ == P - 1)),
                          reads=[("Dg", d2), ("CBv", b)], writes=[("yacc", j)])

            pend = []
            for s_ in range(P):
                b = gcnt[0] % NCB
                gcnt[0] += 1
                head(s_, b)
                pend.append((s_, b))
                if len(pend) > TAIL_SKEW:
                    tail(*pend.pop(0))
                yield
            while pend:
                tail(*pend.pop(0))
            YA = [("yacc", j) for j in range(4)]
            pr.op("dve", lambda e: e.tensor_tensor(out=X[:, :], in0=X[:, :], in1=yacc[:, :], op=ALU.add), reads=[kX] + YA, writes=[kX])
            jb = gcnt[0] % NCB
            gcnt[0] += 1
            pr.op("act", lambda e: e.activation(out=CB[jb][:, 0:D], in_=X[:, :], func=AF.Square, accum_out=sttb[:, 0:1]),
                  reads=[kX], writes=[("CBu", jb), "ssq3"])
            rstd_from_ssq(sttb[:, 0:1], sttb[:, 1:2], D, "ssq3", "rstd3")
            pr.op("dve", lambda e: e.scalar_tensor_tensor(out=H[:, :], in0=X[:, :], scalar=sttb[:, 1:2], op0=ALU.mult, in1=GF[:, :], op1=ALU.mult),
                  reads=[kX, "rstd3", "GF"], writes=[kH])
            out_toks.append(pr.dma("sp", "m", lambda e: e.dma_start(out=out_d[r0:r0 + P, :], in_=H[:, :]), reads=[kH], writes=[]))
            yield

        nfront = 0
        gv_ = conv_gen(jobsV, [(xt[1], "xt1"), (hn[1], "hn1")], 1)
        gf0_ = front(0)
        dv_ = df_ = False
        while not (dv_ and df_):
            if not dv_:
                try:
                    next(gv_)
                except StopIteration:
                    dv_ = True
            if not df_:
                try:
                    next(gf0_)
                    nfront += 1
                except StopIteration:
                    df_ = True
        for nm_ in pr.dpools["m"][0]:
            if pr.dval[nm_] > 0:
                pr._wait("pool", (nm_, pr.dval[nm_]))
        for ti in range(ntiles):
            gb = back(ti)
            gf = front(ti + 1) if ti + 1 < ntiles else None
            done_b, done_f = False, gf is None
            step = 0
            fcount = 0
            while not (done_b and done_f):
                if not done_b:
                    try:
                        next(gb)
                    except StopIteration:
                        done_b = True
                step += 1
                target = nfront + 1 if done_b else (step * nfront) // FRONT_SPAN
                while not done_f and fcount < target:
                    try:
                        next(gf)
                        fcount += 1
                    except StopIteration:
                        done_f = True
                if done_b and not done_f:
                    continue

        for tok in out_toks:
            pr._wait("sp", tok)
        for e_ in ("act", "dve", "pe", "pool"):
            for tok in out_toks[-1:]:
                pr._wait(e_, tok)
    return nc


_NC_CACHE = {}


def _layout_inputs(inp, b):
    f = lambda a: np.ascontiguousarray(a, dtype=np.float32)
    m = {}
    m["x"] = f(inp["x"][b])
    m["c16"] = f(inp["c"][b].reshape(16, P))
    m["w_ada"] = f(inp["w_ada"][0])
    m["b_ada"] = f(inp["b_ada"][0].reshape(1, -1))
    m["g1row"] = f(inp["g_norm_mix"][0].reshape(1, -1))
    m["g2row"] = f(inp["g_norm_ffn"][0].reshape(1, -1))
    m["gf_b"] = f(np.broadcast_to(inp["g_final"].reshape(1, -1), (P, D)))
    m["gv_b"] = f(np.broadcast_to(inp["g_v"][0].reshape(1, -1), (P, 1024)))
    m["w_in"] = f(inp["w_in"][0])
    m["w_out"] = f(inp["w_out"][0])
    m["w_query"] = f(inp["w_query"][0])
    m["cw"] = f(inp["conv_w"][0].reshape(4, 8, P).transpose(2, 1, 0))
    m["cb"] = f(inp["conv_b"][0].reshape(8, P).T)
    m["wa"] = f(inp["w_gate_a"][0])
    m["wi"] = f(inp["w_gate_i"][0])
    m["ba"] = f(inp["b_gate_a"][0].T)
    m["bi"] = f(inp["b_gate_i"][0].T)
    m["lam"] = f(inp["lru_lambda"][0].reshape(8, P).T)
    m["wsT"] = f(inp["w_spatial"][0].transpose(2, 0, 1))
    m["bsrow"] = f(inp["b_spatial"][0].reshape(1, -1))
    m["keysT"] = f(inp["sub_keys"][0].reshape(16, P, P).transpose(2, 0, 1))
    m["expert_u"] = f(inp["expert_u"][0])
    m["expert_v"] = f(inp["expert_v"][0])
    return m


def kernel(**inputs):
    inp = {k: np.asarray(v) for k, v in inputs.items()}
    if "nc" not in _NC_CACHE:
        _NC_CACHE["nc"] = build_program()
    nc = _NC_CACHE["nc"]
    n = 8
    shared = _layout_inputs(inp, 0)
    in_maps = []
    for b in range(n):
        m = dict(shared)
        m["x"] = np.ascontiguousarray(inp["x"][b], dtype=np.float32)
        m["c16"] = np.ascontiguousarray(inp["c"][b].reshape(16, P), dtype=np.float32)
        in_maps.append(m)
    if DEBUG_CORES != 8:
        in_maps = in_maps[:DEBUG_CORES]
        n = DEBUG_CORES
    res = run_bass_kernel_spmd(nc, in_maps, core_ids=list(range(n)))
    out = np.stack([np.asarray(r["out"]) for r in res.results], axis=0)
    if DEBUG_TILES is not None:
        return out.astype(np.float32)
    return out.reshape(8, SEQ, D).astype(np.float32)
```
